# Optimizing a Trainium2 kernel written in Bass

```python
import jax, jax.numpy as jnp
from jax import lax
import numpy as np

D_MODEL = 1024
BATCH = 8
SEQ = 4096
DEPTH = 2

MEM_LEN = 256
POOL_WIDTH = 512
POOL_GROUPS = 4
POOL_WINDOWS = (2, 4, 8, 16)
POOL_GROUP_DIM = POOL_WIDTH // POOL_GROUPS
SGU_WIDTH = 512
SGU_GROUPS = 4
SGU_GROUP_DIM = SGU_WIDTH // SGU_GROUPS
CHUNK = 128
IN_COLS = POOL_WIDTH + 2 * SGU_WIDTH + 2 * D_MODEL
XATTN_HEADS = 4
XATTN_HEAD_DIM = D_MODEL // XATTN_HEADS
D_FF = 2816
N_EXPERTS = 8
TOP_K = 2
D_FF_EXPERT = 3584
N_DENSE = (DEPTH + 1) // 2
N_MOE = DEPTH // 2
EPS = 1e-6

kernel_name = "hybrid_pool_sgu_xattn_moe_trunk"


def rms_norm(x, g):
    xf = x.astype(jnp.float32)
    y = xf * lax.rsqrt(jnp.mean(xf * xf, axis=-1, keepdims=True) + EPS)
    return (y * g.astype(jnp.float32)).astype(x.dtype)


def layer_norm(x, g, b):
    xf = x.astype(jnp.float32)
    mu = jnp.mean(xf, axis=-1, keepdims=True)
    xc = xf - mu
    y = xc * lax.rsqrt(jnp.mean(xc * xc, axis=-1, keepdims=True) + EPS)
    return (y * g.astype(jnp.float32) + b.astype(jnp.float32)).astype(x.dtype)


def pool_mixer(p, w_mix, scale):
    b, s, _ = p.shape
    pf = p.astype(jnp.float32)
    cs = jnp.cumsum(pf, axis=1)
    t = jnp.arange(s)
    outs = []
    for gi, w in enumerate(POOL_WINDOWS):
        sl = slice(gi * POOL_GROUP_DIM, (gi + 1) * POOL_GROUP_DIM)
        c = cs[..., sl]
        lag = jnp.pad(c, ((0, 0), (w, 0), (0, 0)))[:, :s]
        cnt = jnp.minimum(t + 1, w).astype(jnp.float32)[None, :, None]
        outs.append((c - lag) / cnt - pf[..., sl])
    pooled = jnp.stack(outs, axis=2).astype(p.dtype)
    mixed = jnp.einsum("bsgc,gcd->bsgd", pooled, w_mix)
    return mixed.reshape(b, s, POOL_WIDTH) * scale


def spatial_gating(u, v, ln_g, ln_b, w_sp, b_sp):
    b, s, _ = u.shape
    n = s // CHUNK
    v = layer_norm(v, ln_g, ln_b)
    vc = v.reshape(b, n, CHUNK, SGU_GROUPS, SGU_GROUP_DIM)
    mask = jnp.tril(jnp.ones((CHUNK, CHUNK), dtype=bool))
    w = jnp.where(mask, w_sp, jnp.zeros((), w_sp.dtype))
    mixed = jnp.einsum("gts,bnsgc->bntgc", w, vc) + b_sp.T[None, None, :, :, None]
    return u * mixed.reshape(b, s, SGU_WIDTH)


def cross_attention(h, mem_n, w_q, w_kv, w_o):
    b, s, _ = h.shape
    q = (h @ w_q).reshape(b, s, XATTN_HEADS, XATTN_HEAD_DIM)
    kv = (mem_n @ w_kv).reshape(b, MEM_LEN, 2, XATTN_HEADS, XATTN_HEAD_DIM)
    k, v = kv[:, :, 0], kv[:, :, 1]
    sc = jnp.einsum("bshd,bmhd->bhsm", q, k).astype(jnp.float32) * (XATTN_HEAD_DIM ** -0.5)
    pr = jax.nn.softmax(sc, axis=-1).astype(v.dtype)
    o = jnp.einsum("bhsm,bmhd->bshd", pr, v).reshape(b, s, D_MODEL)
    return o @ w_o


def swiglu(h, w_gu, w_down):
    g, up = jnp.split(h @ w_gu, 2, axis=-1)
    return (jax.nn.silu(g) * up) @ w_down


def moe_swiglu(h, w_router, w_gu, w_down):
    logits = (h @ w_router).astype(jnp.float32)
    top_v, top_i = lax.top_k(logits, TOP_K)
    gates = jax.nn.softmax(top_v, axis=-1)
    combine = jnp.sum(jax.nn.one_hot(top_i, N_EXPERTS, dtype=jnp.float32) * gates[..., None], axis=-2)
    combine = combine.astype(h.dtype)
    out = jnp.zeros_like(h)
    for e in range(N_EXPERTS):
        out = out + combine[..., e:e + 1] * swiglu(h, w_gu[e], w_down[e])
    return out


def setup_inputs(seed: int = 0) -> dict:
    key = jax.random.key(seed)
    ks = jax.random.split(key, 32)
    f32 = jnp.float32

    def nrm(k, shape, scale):
        return jax.random.normal(k, shape, f32) * scale

    def gain(k, shape):
        return 1.0 + 0.02 * jax.random.normal(k, shape, f32)

    return {
        "x": nrm(ks[0], (BATCH, SEQ, D_MODEL), 1.0),
        "mem": nrm(ks[1], (BATCH, MEM_LEN, D_MODEL), 1.0),
        "norm_mem": gain(ks[2], (D_MODEL,)),
        "norm_mix": gain(ks[3], (DEPTH, D_MODEL)),
        "w_in": nrm(ks[4], (DEPTH, D_MODEL, IN_COLS), D_MODEL ** -0.5),
        "pool_mix": nrm(ks[5], (DEPTH, POOL_GROUPS, POOL_GROUP_DIM, POOL_GROUP_DIM), POOL_GROUP_DIM ** -0.5),
        "pool_scale": 1.0 + 0.1 * jax.random.normal(ks[6], (DEPTH, POOL_WIDTH), f32),
        "sgu_ln_g": gain(ks[7], (DEPTH, SGU_WIDTH)),
        "sgu_ln_b": nrm(ks[8], (DEPTH, SGU_WIDTH), 0.02),
        "w_spatial": nrm(ks[9], (DEPTH, SGU_GROUPS, CHUNK, CHUNK), CHUNK ** -0.5),
        "b_spatial": 1.0 + 0.1 * jax.random.normal(ks[10], (DEPTH, SGU_GROUPS, CHUNK), f32),
        "w_branch_a": nrm(ks[11], (DEPTH, POOL_WIDTH, D_MODEL), POOL_WIDTH ** -0.5),
        "w_branch_b": nrm(ks[12], (DEPTH, SGU_WIDTH, D_MODEL), SGU_WIDTH ** -0.5),
        "w_out": nrm(ks[13], (DEPTH, D_MODEL, D_MODEL), D_MODEL ** -0.5),
        "norm_xattn": gain(ks[14], (DEPTH, D_MODEL)),
        "w_xq": nrm(ks[15], (DEPTH, D_MODEL, D_MODEL), D_MODEL ** -0.5),
        "w_xkv": nrm(ks[16], (DEPTH, D_MODEL, 2 * D_MODEL), D_MODEL ** -0.5),
        "w_xo": nrm(ks[17], (DEPTH, D_MODEL, D_MODEL), D_MODEL ** -0.5),
        "norm_ffn": gain(ks[18], (DEPTH, D_MODEL)),
        "w_ff_gu": nrm(ks[19], (N_DENSE, D_MODEL, 2 * D_FF), D_MODEL ** -0.5),
        "w_ff_down": nrm(ks[20], (N_DENSE, D_FF, D_MODEL), D_FF ** -0.5),
        "w_router": nrm(ks[21], (N_MOE, D_MODEL, N_EXPERTS), D_MODEL ** -0.5),
        "w_moe_gu": nrm(ks[22], (N_MOE, N_EXPERTS, D_MODEL, 2 * D_FF_EXPERT), D_MODEL ** -0.5),
        "w_moe_down": nrm(ks[23], (N_MOE, N_EXPERTS, D_FF_EXPERT, D_MODEL), D_FF_EXPERT ** -0.5),
        "norm_final": gain(ks[24], (D_MODEL,)),
    }


def reference(x, mem, norm_mem, norm_mix, w_in, pool_mix, pool_scale, sgu_ln_g, sgu_ln_b,
              w_spatial, b_spatial, w_branch_a, w_branch_b, w_out, norm_xattn, w_xq, w_xkv,
              w_xo, norm_ffn, w_ff_gu, w_ff_down, w_router, w_moe_gu, w_moe_down, norm_final):
    splits = [POOL_WIDTH, POOL_WIDTH + SGU_WIDTH, POOL_WIDTH + 2 * SGU_WIDTH,
              POOL_WIDTH + 2 * SGU_WIDTH + D_MODEL]
    mem_n = rms_norm(mem, norm_mem)
    for l in range(DEPTH):
        h = rms_norm(x, norm_mix[l])
        p, u, v, ga, gb = jnp.split(h @ w_in[l], splits, axis=-1)
        br_a = pool_mixer(p, pool_mix[l], pool_scale[l]) @ w_branch_a[l]
        br_b = spatial_gating(jax.nn.gelu(u), jax.nn.gelu(v), sgu_ln_g[l], sgu_ln_b[l],
                              w_spatial[l], b_spatial[l]) @ w_branch_b[l]
        merged = jax.nn.sigmoid(ga) * br_a + jax.nn.sigmoid(gb) * br_b
        x = x + merged @ w_out[l]
        h = rms_norm(x, norm_xattn[l])
        x = x + cross_attention(h, mem_n, w_xq[l], w_xkv[l], w_xo[l])
        h = rms_norm(x, norm_ffn[l])
        if l % 2 == 0:
            x = x + swiglu(h, w_ff_gu[l // 2], w_ff_down[l // 2])
        else:
            x = x + moe_swiglu(h, w_router[l // 2], w_moe_gu[l // 2], w_moe_down[l // 2])
    return rms_norm(x, norm_final)
```

```python
from contextlib import ExitStack
import numpy as np
import concourse.bass as bass
import concourse.mybir as mybir
from concourse.bass_utils import run_bass_kernel_spmd

F32 = mybir.dt.float32
I32 = mybir.dt.int32
BF16 = mybir.dt.bfloat16
AF = mybir.ActivationFunctionType
ALU = mybir.AluOpType
AX = mybir.AxisListType

PE, ACT, DVE, POOL, SP = "pe", "act", "dve", "pool", "sp"
COMPUTE = (PE, ACT, DVE, POOL)

D = 1024
SEQ = 4096
MEM = 256
DEPTH = 2
TT = 512
NT = 2
TP = TT * NT
NPASS = SEQ // TP
DFF = 2816
NE = 8
DFE = 3584
EPS = 1e-6
NS = 12
SLAB = 2048
GSL = 512
NQ = GSL // 128
MAXG = TP // GSL
NSTAGE = 2
SCRF = 8448
SCRB = 20480


class Buf:
    __slots__ = ("name", "last_w", "readers", "dma_sem", "dma_sem_hw")

    def __init__(self, name):
        self.name = name
        self.last_w = None
        self.readers = []
        self.dma_sem = None
        self.dma_sem_hw = None


class Prog:
    def __init__(self, nc, stack, dry=False):
        self.nc = nc
        self.stack = stack
        self.dry = dry
        self.ops = {e: [] for e in (PE, ACT, DVE, POOL, SP)}
        self.sems = {}
        self.cnt = {}
        self.seen = {e: {} for e in self.ops}
        self.n_dma_sems = 0
        self.cstack = []
        if not dry:
            for e in COMPUTE:
                self._mksem(e)
            self.regs = {PE: stack.enter_context(nc.tensor.register("r_pe")),
                         ACT: stack.enter_context(nc.scalar.register("r_act")),
                         DVE: stack.enter_context(nc.vector.register("r_dve"))}

    def _mksem(self, key):
        self.sems[key] = self.stack.enter_context(self.nc.semaphore("s_" + key))
        self.cnt[key] = 0

    def _waits_for(self, eng, reads, writes, extra=()):
        need = {}

        def req(dep):
            if dep is None:
                return
            k, v = dep
            if need.get(k, 0) < v:
                need[k] = v
        for b in reads:
            req(b.last_w)
        for b in writes:
            req(b.last_w)
            for r in b.readers:
                req(r)
        for d in extra:
            req(d)
        out = []
        seen = self.seen[eng]
        for k, v in need.items():
            if k == PE and eng == PE:
                continue
            if seen.get(k, 0) < v:
                seen[k] = v
                out.append((k, v))
        return out

    def op(self, eng, emit, reads=(), writes=(), inc=True):
        if self.dry:
            return
        waits = self._waits_for(eng, reads, writes)
        if inc:
            self.cnt[eng] += 1
            me = (eng, self.cnt[eng])
            self.ops[eng].append(("op", waits, emit, eng, 1))
        else:
            me = (eng, self.cnt[eng] + 1)
            self.ops[eng].append(("op", waits, emit, None, 0))
        for b in reads:
            b.readers.append(me)
        for b in writes:
            b.last_w = me
            b.readers = []

    def dma(self, queue, out_ap, in_ap, reads=(), writes=(), extra=()):
        if self.dry:
            return None
        owner = (list(writes) + list(reads))[0]
        attr = "dma_sem" if queue == POOL else "dma_sem_hw"
        if getattr(owner, attr) is None:
            setattr(owner, attr, "d%d" % self.n_dma_sems)
            self.n_dma_sems += 1
            self._mksem(getattr(owner, attr))
        semkey = getattr(owner, attr)
        waits = self._waits_for(queue, reads, writes, extra=extra)
        self.cnt[semkey] += 16
        me = (semkey, self.cnt[semkey])
        self.ops[queue].append(("op", waits, lambda e: e.dma_start(out=out_ap, in_=in_ap), semkey, 16))
        for b in reads:
            b.readers.append(me)
        for b in writes:
            b.last_w = me
            b.readers = []
        return me

    def barrier(self, extra_bufs=()):
        if self.dry:
            return
        deps = [(e, self.cnt[e]) for e in COMPUTE if self.cnt[e] > 0]
        for e in COMPUTE:
            waits = self._waits_for(e, extra_bufs, extra_bufs, extra=deps)
            if waits:
                self.ops[e].append(("op", waits, None, None, 0))

    def cond_load(self, cnt_ap, cnt_buf):
        if self.dry:
            return
        for e in (PE, ACT, DVE):
            waits = self._waits_for(e, [cnt_buf], [])
            cnt_buf.readers.append((e, self.cnt[e] + 1))
            self.ops[e].append(("cload", waits, cnt_ap))

    def cond_begin(self, cnt_ap, cnt_buf, thresh):
        if self.dry:
            return
        info = {"ap": cnt_ap, "thresh": thresh, "n": {}, "start": {}, "snap": {}, "pos": {}}
        for e in (PE, ACT, DVE):
            waits = []
            if cnt_ap is not None:
                waits = self._waits_for(e, [cnt_buf], [])
                cnt_buf.readers.append((e, self.cnt[e] + 1))
            info["start"][e] = self.cnt[e]
            info["snap"][e] = dict(self.seen[e])
            info["pos"][e] = len(self.ops[e])
            self.ops[e].append(("cbegin", waits, info))
        self.cstack.append(info)

    def cond_end(self):
        if self.dry:
            return
        info = self.cstack.pop()
        for e in (PE, ACT, DVE):
            info["n"][e] = self.cnt[e] - info["start"][e]
            assert info["n"][e] > 0, "conditional block without incrementing op on " + e
            self.seen[e] = info["snap"][e]
            self.ops[e].append(("cend", info))

    def final_wait(self, eng, bufs):
        if self.dry:
            return
        waits = self._waits_for(eng, bufs, bufs)
        self.ops[eng].append(("op", waits, None, None, 0))

    def emit_all(self):
        nc = self.nc
        sems = self.sems
        with nc.Block() as block:
            def run(eng_name):
                def body(e):
                    gstack = []
                    for ent in self.ops[eng_name]:
                        kind = ent[0]
                        if kind == "op":
                            _, waits, emit, semkey, inc = ent
                            for k, v in waits:
                                e.wait_ge(sems[k], v)
                            if emit is not None:
                                ins = emit(e)
                                if inc:
                                    ins.then_inc(sems[semkey], inc)
                        elif kind == "cload":
                            _, waits, cap = ent
                            for k, v in waits:
                                e.wait_ge(sems[k], v)
                            e.reg_load(self.regs[eng_name], cap)
                        elif kind == "cbegin":
                            _, waits, info = ent
                            for k, v in waits:
                                e.wait_ge(sems[k], v)
                            reg = self.regs[eng_name]
                            if info["ap"] is not None:
                                e.reg_load(reg, info["ap"])
                            g = e.If_lt(reg, info["thresh"])
                            g.__enter__()
                            e.drain()
                            e.sem_inc(sems[eng_name], info["n"][eng_name])
                            g.__exit__(None, None, None)
                            g2 = e.Else()
                            g2.__enter__()
                            gstack.append(g2)
                        else:
                            gstack.pop().__exit__(None, None, None)
                return body
            block.tensor(run(PE))
            block.scalar(run(ACT))
            block.vector(run(DVE))
            block.gpsimd(run(POOL))
            block.sync(run(SP))


class Ring:
    def __init__(self, P, ring_t, plan=None, cache=None, n_pass=1):
        self.P = P
        self.ring_t = ring_t
        self.cache = cache
        self.n_pass = n_pass
        self.cache_ready = {}
        self.dry = plan is None
        self.plan = [] if plan is None else plan
        self.idx = 0
        self.next_load = 0
        self.released = [False] * (len(self.plan) if plan else 0)
        self.bufs = [Buf("slab%d" % i) for i in range(NS)]

    def view(self, slot, kc):
        return self.ring_t[:, slot, :].rearrange("p (kc n) -> p kc n", kc=kc)

    def _pump(self):
        while self.next_load < len(self.plan):
            j = self.next_load
            if j >= NS and not self.released[j - NS]:
                break
            src, kc = self.plan[j]
            slot = j % NS
            npp = len(self.plan) // self.n_pass
            if self.cache is None or self.n_pass == 1:
                self.P.dma(POOL, self.view(slot, kc), src, writes=[self.bufs[slot]])
            elif j < npp:
                self.P.dma(POOL, self.view(slot, kc), src, writes=[self.bufs[slot]])
                cview = self.cache[j].rearrange("p (kc n) -> p kc n", kc=kc)
                self.cache_ready[j] = self.P.dma(SP, cview, self.view(slot, kc), reads=[self.bufs[slot]])
            else:
                jj = j % npp
                assert self.plan[jj][1] == kc
                cview = self.cache[jj].rearrange("p (kc n) -> p kc n", kc=kc)
                self.P.dma(SP, self.view(slot, kc), cview, writes=[self.bufs[slot]], extra=[self.cache_ready[jj]])
            self.next_load += 1

    def get(self, src, kc):
        j = self.idx
        self.idx += 1
        if self.dry:
            self.plan.append((src, kc))
            return j, self.view(0, kc), self.bufs[0]
        self._pump()
        assert self.next_load > j, "ring too small for simultaneous residency (load %d)" % j
        slot = j % NS
        return j, self.view(slot, kc), self.bufs[slot]

    def done(self, j):
        if self.dry:
            return
        self.released[j] = True
        self._pump()


class WBlk:
    def __init__(self, R, w_ap, c0, ncols):
        self.R = R
        self.ncols = ncols
        if ncols == 512:
            self.sl = [R.get(wsrc(w_ap, h * 512, 512, c0, 512), 4) for h in range(2)]
        else:
            assert ncols == 256
            self.sl = [R.get(wsrc(w_ap, 0, D, c0, 256), 8)]

    def lhsT(self, k, c):
        if self.ncols == 512:
            s = self.sl[k // 4]
            return s[1][:, k % 4, c * 128:(c + 1) * 128], s[2]
        s = self.sl[0]
        return s[1][:, k, c * 128:(c + 1) * 128], s[2]

    def rows(self, k, c0, n):
        if self.ncols == 512:
            s = self.sl[k // 4]
            return s[1][:, k % 4, c0:c0 + n], s[2]
        s = self.sl[0]
        return s[1][:, k, c0:c0 + n], s[2]

    def done(self):
        for s in self.sl:
            self.R.done(s[0])


def wsrc(w_ap, r0, nr, c0, ncol):
    return w_ap[r0:r0 + nr, c0:c0 + ncol].rearrange("(kc p) n -> p kc n", p=128)


class Builder:
    def __init__(self, nc, st, n_pass=NPASS, stop_after=None):
        self.nc = nc
        self.st = st
        self.n_pass = n_pass
        self.stop_after = stop_after
        self.declare_dram()
        self.alloc()

    def declare_dram(self):
        nc = self.nc

        def inp(name, shape):
            return nc.dram_tensor(name, list(shape), F32, kind="ExternalInput").ap()
        self.xT = inp("xT", [D, SEQ])
        self.memT = inp("memT", [D, MEM])
        self.vecs = inp("vecs", [128, NVEC])
        self.bcs = inp("bcs", [DEPTH, 128, 1536])
        self.cst = inp("cst", [128, 960])
        self.w_in = inp("w_in", [DEPTH, D, 3584])
        self.pool_mix = inp("pool_mix", [DEPTH, 128, 4, 128])
        self.wspT = inp("wspT", [DEPTH, 128, 4, 128])
        self.w_bra = inp("w_branch_a", [DEPTH, 512, D])
        self.w_brb = inp("w_branch_b", [DEPTH, 512, D])
        self.w_out = inp("w_out", [DEPTH, D, D])
        self.w_xq = inp("w_xq", [DEPTH, D, D])
        self.w_xkv = inp("w_xkv", [DEPTH, D, 2 * D])
        self.w_xo = inp("w_xo", [DEPTH, D, D])
        self.w_ff_gu = inp("w_ff_gu", [1, D, 2 * DFF])
        self.w_ff_down = inp("w_ff_down", [1, DFF, D])
        self.w_router = inp("w_router", [128, 8, NE])
        self.w_moe_gu = inp("w_moe_gu", [1, NE, D, 2 * DFE])
        self.w_moe_down = inp("w_moe_down", [1, NE, DFE, D])
        self.outT = nc.dram_tensor("outT", [D, SEQ], F32, kind="ExternalOutput").ap()

    def sb(self, name, shape, dt):
        return self.st.enter_context(self.nc.sbuf_tensor(name, list(shape), dt))

    def alloc(self):
        nc = self.nc
        self.x = self.sb("x", [128, NT, 8, TT], F32)
        self.bx = [[Buf("x%d_%d" % (t, k)) for k in range(8)] for t in range(NT)]
        self.h = self.sb("h", [128, NT, 8, TT], BF16)
        self.bh = [Buf("h%d" % t) for t in range(NT)]
        self.mg = self.sb("mg", [128, NT, 8, TT], BF16)
        self.bmg = [[Buf("mg%d_%d" % (t, k)) for k in range(8)] for t in range(NT)]
        self.memn = self.sb("memn", [128, 8, MEM], BF16)
        self.bmemn = Buf("memn")
        self.ring_t = self.sb("ring", [128, NS, SLAB], BF16)
        self.vec_t = self.sb("vec_t", [128, NVEC], F32)
        self.bvec = Buf("vec")
        self.bc_t = self.sb("bc_t", [128, 1536], F32)
        self.bbc = Buf("bc")
        self.cst_t = self.sb("cst_t", [128, 960], F32)
        self.bcst = Buf("cst")
        self.ones_bf = self.sb("ones_bf", [128, 128], BF16)
        self.bones = Buf("ones")
        self.pmix = self.sb("pmix", [128, 4, 128], BF16)
        self.bpmix = Buf("pmix")
        self.wsp_f = self.sb("wsp_f", [128, 4, 128], F32)
        self.bwspf = Buf("wspf")
        self.wsp = self.sb("wsp", [128, 4, 128], BF16)
        self.bwsp = Buf("wsp")
        self.wr = self.sb("wr", [128, 8, NE], F32)
        self.bwr = Buf("wr")
        self.halo = self.sb("halo", [128, DEPTH, 4, 16], F32)
        self.bhalo = [Buf("halo%d" % l) for l in range(DEPTH)]
        self.bmemf = Buf("memf")
        self.U_bf = self.sb("U_bf", [128, 128], BF16)
        self.bU = Buf("U")
        self.comb_t = self.sb("comb_t", [128, 8, NE], F32)
        self.sel_t = self.sb("sel_t", [128, 8, NE], F32)
        self.selb_t = self.sb("selb_t", [128, 8, NE], BF16)
        self.posm_t = self.sb("posm_t", [128, 8, NE], F32)
        self.cntf_t = self.sb("cntf_t", [128, NE], F32)
        self.cnti_t = self.sb("cnti_t", [1, NE], I32)
        self.scr_f = self.sb("scr_f", [128, SCRF], F32)
        self.scr_b = self.sb("scr_b", [128, SCRB], BF16)
        self.psum = [self.st.enter_context(nc.psum_tensor("ps%d" % i, [128, 512], F32)) for i in range(8)]
        self.bps = [Buf("ps%d" % i) for i in range(8)]
        self.ps_i = 0

    def next_ps(self):
        i = self.ps_i % 8
        self.ps_i += 1
        return self.psum[i], self.bps[i]

    def mm(self, out, lhsT, rhs, start, stop, reads, writes, inc=None):
        self.P.op(PE, lambda e: e.matmul(out, lhsT=lhsT, rhs=rhs, start=start, stop=stop), reads, writes,
                  inc=(stop if inc is None else inc))

    def act(self, out, in_, func, reads, writes, **kw):
        self.P.op(ACT, lambda e: e.activation(out=out, in_=in_, func=func, **kw), reads, writes)

    def tt(self, eng, out, in0, in1, op, reads, writes):
        self.P.op(eng, lambda e: e.tensor_tensor(out=out, in0=in0, in1=in1, op=op), reads, writes)

    def ts(self, eng, out, in0, s1, s2, op0, op1, reads, writes):
        if op1 is None:
            s2, op1 = 0.0, ALU.add
        self.P.op(eng, lambda e: e.tensor_scalar(out=out, in0=in0, scalar1=s1, scalar2=s2, op0=op0, op1=op1), reads, writes)

    def stt(self, eng, out, in0, scalar, in1, op0, op1, reads, writes):
        self.P.op(eng, lambda e: e.scalar_tensor_tensor(out=out, in0=in0, scalar=scalar, in1=in1, op0=op0, op1=op1), reads, writes)

    def cp(self, eng, out, in_, reads, writes):
        self.P.op(eng, lambda e: e.tensor_copy(out=out, in_=in_), reads, writes)

    def scratch(self, phase):
        f, b = self.scr_f, self.scr_b
        s = {}

        def fv(name, off, shape):
            n = int(np.prod(shape))
            ap = f[:, off:off + n]
            if len(shape) == 2:
                ap = ap.rearrange("p (a b) -> p a b", a=shape[0])
            return ap, off + n

        def bv(name, off, shape):
            n = int(np.prod(shape))
            ap = b[:, off:off + n]
            if len(shape) == 2:
                ap = ap.rearrange("p (a b) -> p a b", a=shape[0])
            return ap, off + n
        of = 0
        ob = 0
        self.rstd_a, of = fv("rstd_a", of, [TT])
        self.rstd, of = fv("rstd", of, [TT])
        self.sq, ob = bv("sq", ob, [8, TT])
        self.brstd_a, self.brstd, self.bsq = Buf("rstd_a"), Buf("rstd"), Buf("sq")
        self.bsqk = [Buf("sq%d" % k) for k in range(8)]
        if phase == "base":
            self.memf, of = fv("memf", of, [8, MEM])
        elif phase == "mix":
            self.pext, of = fv("pext", of, [4, 528])
            self.bpext = [Buf("pext%d" % c) for c in range(4)]
            self.pltmp, of = fv("pltmp", of, [2, 528])
            self.bpltmp = [Buf("pltmp%d" % i) for i in range(2)]
            self.vtm, of = fv("vtm", of, [4, TT])
            self.bvtm = [Buf("vtm%d" % i) for i in range(4)]
            self.tmpf, of = fv("tmpf", of, [2, TT])
            self.btmpf = [Buf("tmpf%d" % i) for i in range(2)]
            self.stat, of = fv("stat", of, [4, 16])
            self.bstat = [Buf("stat%d" % i) for i in range(4)]
            self.pooled, ob = bv("pooled", ob, [4, TT])
            self.bpooled = [Buf("pooled%d" % c) for c in range(4)]
            self.mixed, ob = bv("mixed", ob, [4, TT])
            self.bmixed = Buf("mixed")
            self.sig, ob = bv("sig", ob, [8, TT])
            self.bsig = [Buf("sig%d" % i) for i in range(8)]
            self.gu, ob = bv("gu", ob, [4, TT])
            self.bgu = [Buf("gu%d" % c) for c in range(4)]
            self.vn, ob = bv("vn", ob, [4, TT])
            self.bvn = [Buf("vn%d" % j) for j in range(4)]
            self.sgu, ob = bv("sgu", ob, [4, TT])
            self.bsgu = Buf("sgu")
        elif phase == "xat":
            self.rs, of = fv("rs", of, [2, TT])
            self.brs = [Buf("rs%d" % i) for i in range(2)]
            self.KT, ob = bv("KT", ob, [8, MEM])
            self.bKT = Buf("KT")
            self.V, ob = bv("V", ob, [2, D])
            self.bV = Buf("V")
            self.eT, ob = bv("eT", ob, [4, TT])
            self.beT = [Buf("eT%d" % i) for i in range(2)]
        elif phase == "ffn":
            self.hf, of = fv("hf", of, [8, TT])
            self.bhf = Buf("hf")
            self.sg, of = fv("sg", of, [2, TT])
            self.bsg = [Buf("sg%d" % i) for i in range(2)]
            self.cbc, of = fv("cbc", of, [2, TT])
            self.bcbc = [Buf("cbc%d" % i) for i in range(2)]
            self.cexp, of = fv("cexp", of, [2, 128])
            self.bcexp = [Buf("cexp%d" % i) for i in range(2)]
            self.comb, of = fv("comb", of, [8, NE])
            self.bcomb = [Buf("comb%d" % j) for j in range(8)]
            self.rt, of = fv("rt", of, [8, 16])
            self.brt = [Buf("rt%d" % i) for i in range(2)]
            self.actb, ob = bv("actb", ob, [8, TT])
            self.bact = [Buf("act%d" % i) for i in range(2)]
        elif phase == "moe_a":
            self.hf, of = fv("hf", of, [8, TT])
            self.bhf = Buf("hf")
            self.rt, of = fv("rt", of, [8, 16])
            self.brt = [Buf("rt%d" % i) for i in range(2)]
        elif phase == "moe_b":
            of = 0
            ob = 0
            yacc_lo, of = fv("yacc", of, [NQ, D])
            yacc_hi = self.h[:].rearrange("p t k n -> p (t k n)").bitcast(F32).rearrange("p (a b) -> p a b", a=NQ)
            self.yacc_parts = [yacc_lo, yacc_hi]
            assert MAXG == 2 and NQ == 4
            self.byacc = [[Buf("yacc%d_%d" % (i, q)) for q in range(NQ)] for i in range(MAXG)]
            self.combT, of = fv("combT", of, [TP])
            self.bcombT = Buf("combT")
            self.posT, of = fv("posT", of, [TP])
            self.bposT = Buf("posT")
            self.sg, of = fv("sg", of, [2, GSL])
            self.bsg = [Buf("sg%d" % i) for i in range(2)]
            self.cexp, of = fv("cexp", of, [2, 128])
            self.bcexp = [Buf("cexp%d" % i) for i in range(2)]
            self.stmp, of = fv("stmp", of, [2, TT])
            self.bstmp = [Buf("stmp%d" % i) for i in range(2)]
            self.actb, ob = bv("actb", ob, [8, GSL])
            self.bact = [Buf("act%d" % i) for i in range(2)]
            self.selbuf, ob = bv("selbuf", ob, [8 * GSL])
            self.bsel = Buf("selbuf")
            self.yb, ob = bv("yb", ob, [NQ, D])
            self.byb = [Buf("yb%d" % q) for q in range(NQ)]
            self.he, ob = bv("he", ob, [MAXG * 8, GSL])
            self.bhe = [Buf("he%d" % i) for i in range(MAXG)]
        elif phase == "fin":
            self.ofin, of = fv("ofin", of, [8, TT])
        assert of <= SCRF and ob <= SCRB, (phase, of, ob)

    def vcol(self, c0, n=1):
        return self.vec_t[:, c0:c0 + n]

    def rmsnorm(self, xin, bxin, gcol, out_bf, bout, n=TT, out_f32=None, bout_f32=None):
        for kc in range(8):
            if kc % 2 == 0:
                self.act(self.sq[:, kc, 0:n], xin(kc), AF.Square, [bxin(kc)], [self.bsqk[kc]])
            else:
                self.tt(DVE, self.sq[:, kc, 0:n], xin(kc), xin(kc), ALU.mult, [bxin(kc)], [self.bsqk[kc]])
        ps, bps = self.next_ps()
        for kc in range(8):
            self.mm(ps[:, 0:n], self.ones_bf[:], self.sq[:, kc, 0:n], kc == 0, kc == 7, [self.bones, self.bsqk[kc]], [bps])
        self.act(self.rstd_a[:, 0:n], ps[:, 0:n], AF.Sqrt, [bps], [self.brstd_a], bias=EPS, scale=1.0 / D)
        self.P.op(DVE, lambda e: e.reciprocal(out=self.rstd[:, 0:n], in_=self.rstd_a[:, 0:n]), [self.brstd_a], [self.brstd])
        for kc in range(8):
            if out_bf is not None:
                self.stt(DVE, out_bf(kc), xin(kc), self.vcol(gcol + kc), self.rstd[:, 0:n], ALU.mult, ALU.mult,
                         [bxin(kc), self.bvec, self.brstd], [bout])
            if out_f32 is not None:
                self.stt(DVE, out_f32(kc), xin(kc), self.vcol(gcol + kc), self.rstd[:, 0:n], ALU.mult, ALU.mult,
                         [bxin(kc), self.bvec, self.brstd], [bout_f32])

    def xin_t(self, t):
        return (lambda kc: self.x[:, t, kc, :]), (lambda kc: self.bx[t][kc])

    def proj_accum_x(self, t, blks, src_chunks, bsrc, kchunks):
        n = 0
        for wb in blks:
            for c in range(wb.ncols // 128):
                ps, bps = self.next_ps()
                for k in range(kchunks):
                    wl, wbuf = wb.lhsT(k, c)
                    self.mm(ps[:], wl, src_chunks(k), k == 0, k == kchunks - 1, [wbuf] + bsrc, [bps])
                self.tt(DVE, self.x[:, t, n, :], self.x[:, t, n, :], ps[:], ALU.add, [bps, self.bx[t][n]], [self.bx[t][n]])
                n += 1

    def load_consts(self):
        P = self.P
        P.dma(SP, self.vec_t[:], self.vecs, writes=[self.bvec])
        P.dma(SP, self.cst_t[:], self.cst, writes=[self.bcst])
        P.dma(SP, self.wr[:], self.w_router, writes=[self.bwr])
        P.op(DVE, lambda e: e.memset(self.ones_bf[:], 1.0), [], [self.bones])
        P.op(DVE, lambda e: e.memset(self.halo[:], 0.0), [], self.bhalo)
        self.tt(DVE, self.U_bf[:], self.mask(), self.ident(), ALU.subtract, [self.bcst], [self.bU])

    def ident(self):
        return self.cst_t[:, 0:128]

    def mask(self):
        return self.cst_t[:, 128:256]

    def iota_row(self):
        return self.cst_t[:, 320:320 + GSL]

    def iota_col(self):
        return self.cst_t[:, 832:833]

    def ratio(self, c):
        return self.cst_t[:, 256 + c * 16:256 + (c + 1) * 16]

    def compute_memn(self):
        self.scratch("base")
        self.P.dma(SP, self.memf, self.memT.rearrange("(kc p) m -> p kc m", p=128), writes=[self.bmemf])
        self.rmsnorm(lambda kc: self.memf[:, kc, :], lambda kc: self.bmemf, V_NMEM,
                     lambda kc: self.memn[:, kc, :], self.bmemn, n=MEM)

    def load_layer_consts(self, l):
        P = self.P
        P.dma(SP, self.bc_t[:], self.bcs[l], writes=[self.bbc])
        P.dma(POOL, self.pmix[:], self.pool_mix[l], writes=[self.bpmix])
        P.dma(SP, self.wsp_f[:], self.wspT[l], writes=[self.bwspf])
        for g in range(4):
            self.tt(DVE, self.wsp[:, g, :], self.wsp_f[:, g, :], self.mask(), ALU.mult, [self.bwspf, self.bcst], [self.bwsp])

    def phase_mixer(self, l, p):
        P, R = self.P, self.R
        P.barrier(extra_bufs=[self.bofin])
        self.scratch("mix")
        win = self.w_in[l]
        for t in range(NT):
            xi, bxi = self.xin_t(t)
            self.rmsnorm(xi, bxi, V_NMIX + 8 * l, lambda kc, t=t: self.h[:, t, kc, :], self.bh[t])
        wb_p = WBlk(R, win, 0, 512)
        wb_ga = [WBlk(R, win, 1536 + c * 512, 512) for c in range(2)]
        sl_bra = [R.get(wsrc(self.w_bra[l], 0, 512, c * 512, 512), 4) for c in range(2)]
        for t in range(NT):
            gt = p * NT + t
            hk = lambda k, t=t: self.h[:, t, k, :]
            for c in range(4):
                ps, bps = self.next_ps()
                for k in range(8):
                    wl, wbuf = wb_p.lhsT(k, c)
                    self.mm(ps[:], wl, hk(k), k == 0, k == 7, [wbuf, self.bh[t]], [bps])
                self.act(self.pext[:, c, 16:528], ps[:], AF.Copy, [bps], [self.bpext[c]])
                if gt == 0:
                    self.P.op(DVE, lambda e, c=c: e.memset(self.pext[:, c, 0:16], 0.0), [], [self.bpext[c]])
                else:
                    self.cp(DVE, self.pext[:, c, 0:16], self.halo[:, l, c, :], [self.bhalo[l]], [self.bpext[c]])
            for c in range(4):
                self.cp(DVE, self.halo[:, l, c, :], self.pext[:, c, 512:528], [self.bpext[c]], [self.bhalo[l]])
            for n in range(8):
                ps, bps = self.next_ps()
                for k in range(8):
                    wl, wbuf = wb_ga[n // 4].lhsT(k, n % 4)
                    self.mm(ps[:], wl, hk(k), k == 0, k == 7, [wbuf, self.bh[t]], [bps])
                self.act(self.sig[:, n, :], ps[:], AF.Sigmoid, [bps], [self.bsig[n]])
            for c in range(4):
                w = 2 << c
                cur, bcur = self.pext[:, c, :], self.bpext[c]
                for k in range(c + 1):
                    sh = 1 << k
                    lo = (2 << k) - 1
                    nxt, bnxt = self.pltmp[:, k % 2, :], self.bpltmp[k % 2]
                    self.tt(DVE, nxt[:, lo:528], cur[:, lo:528], cur[:, lo - sh:528 - sh], ALU.add, [bcur], [bnxt])
                    cur, bcur = nxt, bnxt
                if gt == 0:
                    self.tt(DVE, cur[:, 16:32], cur[:, 16:32], self.ratio(c), ALU.mult, [bcur, self.bcst], [bcur])
                self.stt(DVE, self.pooled[:, c, :], cur[:, 16:528], 1.0 / w, self.pext[:, c, 16:528], ALU.mult, ALU.subtract,
                         [bcur, self.bpext[c]], [self.bpooled[c]])
            for c in range(4):
                ps, bps = self.next_ps()
                self.mm(ps[:], self.pmix[:, c, :], self.pooled[:, c, :], True, True, [self.bpmix, self.bpooled[c]], [bps])
                self.act(self.mixed[:, c, :], ps[:], AF.Identity, [bps, self.bvec], [self.bmixed], scale=self.vcol(V_PSC + 4 * l + c))
            for n in range(8):
                j2, sv2, sbuf2 = sl_bra[n // 4]
                ps2, bps2 = self.next_ps()
                for k in range(4):
                    self.mm(ps2[:], sv2[:, k, (n % 4) * 128:(n % 4 + 1) * 128], self.mixed[:, k, :], k == 0, k == 3,
                            [sbuf2, self.bmixed], [bps2])
                self.tt(DVE, self.mg[:, t, n, :], ps2[:], self.sig[:, n, :], ALU.mult, [bps2, self.bsig[n]], [self.bmg[t][n]])
        for wb in [wb_p] + wb_ga:
            wb.done()
        for s in sl_bra:
            R.done(s[0])
        if self.stop_after == ("mixA", l):
            return
        wb_u = WBlk(R, win, 512, 512)
        wb_v = WBlk(R, win, 1024, 512)
        wb_gb = [WBlk(R, win, 2560 + c * 512, 512) for c in range(2)]
        sl_brb = [R.get(wsrc(self.w_brb[l], 0, 512, c * 512, 512), 4) for c in range(2)]
        for t in range(NT):
            hk = lambda k, t=t: self.h[:, t, k, :]
            for jc in range(4):
                vt, bvt = self.vtm[:, jc, :], self.bvtm[jc]
                for half in range(2):
                    ps, bps = self.next_ps()
                    for k in range(8):
                        wr_, wbuf = wb_v.rows(k, half * 256, 256)
                        self.mm(ps[:, 0:256], self.h[:, t, k, jc * 128:(jc + 1) * 128], wr_, k == 0, k == 7,
                                [wbuf, self.bh[t]], [bps])
                    self.act(vt[:, half * 256:(half + 1) * 256], ps[:, 0:256], AF.Gelu_apprx_tanh, [bps], [bvt])
            for c in range(4):
                ps, bps = self.next_ps()
                for k in range(8):
                    wl, wbuf = wb_u.lhsT(k, c)
                    self.mm(ps[:], wl, hk(k), k == 0, k == 7, [wbuf, self.bh[t]], [bps])
                self.act(self.gu[:, c, :], ps[:], AF.Gelu_apprx_tanh, [bps], [self.bgu[c]])
            for jc in range(4):
                vt, bvt = self.vtm[:, jc, :], self.bvtm[jc]
                st_, bst = self.stat[:, jc, :], self.bstat[jc]
                self.P.op(DVE, lambda e, vt=vt, st_=st_: e.bn_stats(out=st_[:, 0:6], in_=vt), [bvt], [bst])
                self.P.op(DVE, lambda e, st_=st_: e.bn_aggr(out=st_[:, 6:8], in_=st_[:, 0:6]), [bst], [bst])
                self.act(st_[:, 8:9], st_[:, 7:8], AF.Sqrt, [bst], [bst], bias=EPS, scale=1.0)
                self.P.op(DVE, lambda e, st_=st_: e.reciprocal(out=st_[:, 9:10], in_=st_[:, 8:9]), [bst], [bst])
                self.ts(DVE, vt, vt, st_[:, 6:7], st_[:, 9:10], ALU.subtract, ALU.mult, [bvt, bst], [bvt])
                self.tt(DVE, vt, vt, self.bc_t[:, 0:512], ALU.mult, [bvt, self.bbc], [bvt])
                self.tt(DVE, self.vn[:, jc, :], vt, self.bc_t[:, 512:1024], ALU.add, [bvt, self.bbc], [self.bvn[jc]])
            for n in range(8):
                ps, bps = self.next_ps()
                for k in range(8):
                    wl, wbuf = wb_gb[n // 4].lhsT(k, n % 4)
                    self.mm(ps[:], wl, hk(k), k == 0, k == 7, [wbuf, self.bh[t]], [bps])
                self.act(self.sig[:, n, :], ps[:], AF.Sigmoid, [bps], [self.bsig[n]])
            for g in range(4):
                ps, bps = self.next_ps()
                for jc in range(4):
                    self.mm(ps[:, jc * 128:(jc + 1) * 128], self.vn[:, jc, g * 128:(g + 1) * 128], self.wsp[:, g, :], True, True,
                            [self.bvn[jc], self.bwsp], [bps], inc=(jc == 3))
                tf, btf = self.tmpf[:, g % 2, :], self.btmpf[g % 2]
                for jc in range(4):
                    self.tt(DVE, tf[:, jc * 128:(jc + 1) * 128], ps[:, jc * 128:(jc + 1) * 128],
                            self.bc_t[:, 1024 + g * 128:1024 + (g + 1) * 128], ALU.add, [bps, self.bbc], [btf])
                self.tt(DVE, self.sgu[:, g, :], tf, self.gu[:, g, :], ALU.mult, [btf, self.bgu[g]], [self.bsgu])
            for n in range(8):
                j2, sv2, sbuf2 = sl_brb[n // 4]
                ps2, bps2 = self.next_ps()
                for k in range(4):
                    self.mm(ps2[:], sv2[:, k, (n % 4) * 128:(n % 4 + 1) * 128], self.sgu[:, k, :], k == 0, k == 3,
                            [sbuf2, self.bsgu], [bps2])
                tf, btf = self.tmpf[:, n % 2, :], self.btmpf[n % 2]
                self.tt(DVE, tf, ps2[:], self.sig[:, n, :], ALU.mult, [bps2, self.bsig[n]], [btf])
                self.tt(DVE, self.mg[:, t, n, :], self.mg[:, t, n, :], tf, ALU.add, [btf, self.bmg[t][n]], [self.bmg[t][n]])
        for wb in [wb_u, wb_v] + wb_gb:
            wb.done()
        for s in sl_brb:
            R.done(s[0])
        if self.stop_after == ("mixB", l):
            return
        wb_o = [WBlk(R, self.w_out[l], c * 512, 512) for c in range(2)]
        for t in range(NT):
            self.proj_accum_x(t, wb_o, lambda k, t=t: self.mg[:, t, k, :], [self.bmg[t][k] for k in range(8)], 8)
        for wb in wb_o:
            wb.done()

    def phase_xattn(self, l, p):
        P, R = self.P, self.R
        P.barrier(extra_bufs=[self.bofin])
        self.scratch("xat")
        wkv = self.w_xkv[l]
        for c2 in range(2):
            wb = WBlk(R, wkv, c2 * 512, 512)
            for c in range(4):
                n = c2 * 4 + c
                ps, bps = self.next_ps()
                for k in range(8):
                    wl, wbuf = wb.lhsT(k, c)
                    self.mm(ps[:, 0:MEM], wl, self.memn[:, k, :], k == 0, k == 7, [wbuf, self.bmemn], [bps])
                self.act(self.KT[:, n, :], ps[:, 0:MEM], AF.Copy, [bps], [self.bKT])
            wb.done()
        for c2 in range(2):
            wb = WBlk(R, wkv, D + c2 * 512, 512)
            for mc in range(2):
                ps, bps = self.next_ps()
                for k in range(8):
                    wr_, wbuf = wb.rows(k, 0, 512)
                    self.mm(ps[:], self.memn[:, k, mc * 128:(mc + 1) * 128], wr_, k == 0, k == 7, [wbuf, self.bmemn], [bps])
                self.act(self.V[:, mc, c2 * 512:(c2 + 1) * 512], ps[:], AF.Copy, [bps], [self.bV])
            wb.done()
        for t in range(NT):
            xi, bxi = self.xin_t(t)
            self.rmsnorm(xi, bxi, V_NXAT + 8 * l, lambda kc, t=t: self.h[:, t, kc, :], self.bh[t])
        wb_q = [WBlk(R, self.w_xq[l], c * 512, 512) for c in range(2)]
        for t in range(NT):
            for n in range(8):
                ps, bps = self.next_ps()
                for k in range(8):
                    wl, wbuf = wb_q[n // 4].lhsT(k, n % 4)
                    self.mm(ps[:], wl, self.h[:, t, k, :], k == 0, k == 7, [wbuf, self.bh[t]], [bps])
                self.act(self.mg[:, t, n, :], ps[:], AF.Identity, [bps], [self.bmg[t][n]], scale=1.0 / 16.0)
        for wb in wb_q:
            wb.done()
        for t in range(NT):
            for hd in range(4):
                r = hd % 2
                e_, be = self.eT[:, 2 * r:2 * r + 2, :], self.beT[r]
                for mc in range(2):
                    ps, bps = self.next_ps()
                    for dc in range(2):
                        n = hd * 2 + dc
                        self.mm(ps[:], self.KT[:, n, mc * 128:(mc + 1) * 128], self.mg[:, t, n, :], dc == 0, dc == 1,
                                [self.bKT, self.bmg[t][n]], [bps])
                    self.act(e_[:, mc, :], ps[:], AF.Exp, [bps], [be])
                ps, bps = self.next_ps()
                for mc in range(2):
                    self.mm(ps[:], self.ones_bf[:], e_[:, mc, :], mc == 0, mc == 1, [self.bones, be], [bps])
                rs, brs = self.rs[:, r, :], self.brs[r]
                self.P.op(DVE, lambda e, rs=rs, ps=ps: e.reciprocal(out=rs, in_=ps[:]), [bps], [brs])
                for dc in range(2):
                    n = hd * 2 + dc
                    ps, bps = self.next_ps()
                    for mc in range(2):
                        self.mm(ps[:], self.V[:, mc, n * 128:(n + 1) * 128], e_[:, mc, :], mc == 0, mc == 1, [self.bV, be], [bps])
                    self.tt(DVE, self.h[:, t, n, :], ps[:], rs, ALU.mult, [bps, brs], [self.bh[t]])
        if self.stop_after == ("xatO", l):
            return
        wb_o = [WBlk(R, self.w_xo[l], c * 512, 512) for c in range(2)]
        for t in range(NT):
            self.proj_accum_x(t, wb_o, lambda k, t=t: self.h[:, t, k, :], [self.bh[t]], 8)
        for wb in wb_o:
            wb.done()

    def router(self, t):
        for jc in range(4):
            j8 = t * 4 + jc
            r = jc % 2
            rt, brt = self.rt[:, 4 * r:4 * r + 4, :], self.brt[r]
            ps, bps = self.next_ps()
            for k in range(8):
                self.mm(ps[:, 0:NE], self.hf[:, k, jc * 128:(jc + 1) * 128], self.wr[:, k, :], k == 0, k == 7,
                        [self.bhf, self.bwr], [bps])
            lg = rt[:, 0, 0:8]
            self.cp(DVE, lg, ps[:, 0:NE], [bps], [brt])
            m1 = rt[:, 1, 0:1]
            self.P.op(DVE, lambda e, m1=m1, lg=lg: e.reduce_max(out=m1, in_=lg, axis=AX.X), [brt], [brt])
            eq = rt[:, 0, 8:16]
            self.ts(DVE, eq, lg, m1, None, ALU.is_equal, None, [brt], [brt])
            l2 = rt[:, 2, 0:8]
            self.stt(DVE, l2, eq, -1e30, lg, ALU.mult, ALU.add, [brt], [brt])
            m2 = rt[:, 1, 1:2]
            self.P.op(DVE, lambda e, m2=m2, l2=l2: e.reduce_max(out=m2, in_=l2, axis=AX.X), [brt], [brt])
            sel = self.sel_t[:, j8, :]
            self.ts(DVE, sel, lg, m2, None, ALU.is_ge, None, [brt], [brt, self.bselt[j8]])
            self.cp(DVE, self.selb_t[:, j8, :], sel, [self.bselt[j8]], [self.bselb[j8]])
            nm1 = rt[:, 1, 2:3]
            self.ts(DVE, nm1, m1, -1.0, None, ALU.mult, None, [brt], [brt])
            ex = rt[:, 3, 0:8]
            self.act(ex, lg, AF.Exp, [brt], [brt], bias=nm1, scale=1.0)
            exs = rt[:, 3, 8:16]
            self.tt(DVE, exs, ex, sel, ALU.mult, [brt, self.bselt[j8]], [brt])
            den = rt[:, 1, 3:4]
            self.P.op(DVE, lambda e, den=den, exs=exs: e.reduce_sum(out=den, in_=exs, axis=AX.X), [brt], [brt])
            rden = rt[:, 1, 4:5]
            self.P.op(DVE, lambda e, rden=rden, den=den: e.reciprocal(out=rden, in_=den), [brt], [brt])
            self.ts(DVE, self.comb_t[:, j8, :], exs, rden, None, ALU.mult, None, [brt], [self.bcomb[j8]])

    def ffn_expert(self, w_gu, w_down, dff, e=None):
        R = self.R
        g0 = 0
        gi = 0
        while g0 < dff:
            G = min(512, dff - g0)
            nch = G // 128
            wb_g = WBlk(R, w_gu, g0, G)
            wb_u = WBlk(R, w_gu, dff + g0, G)
            sl_d = [R.get(wsrc(w_down, g0, G, c * (SLAB // nch), SLAB // nch), nch) for c in range(D * nch // SLAB)]
            dcols = SLAB // nch
            for t in range(NT):
                a_i = (gi * NT + t) % 2
                actv, bact = self.actb[:, 4 * a_i:4 * a_i + 4, :], self.bact[a_i]
                if e is not None and gi == 0:
                    cb, bcb = self.cbc[:, t, :], self.bcbc[t]
                    ps, bps = self.next_ps()
                    for jc in range(4):
                        j8 = t * 4 + jc
                        cx, bcx = self.cexp[:, jc % 2, :], self.bcexp[jc % 2]
                        self.cp(DVE, cx, self.comb[:, j8, e:e + 1].to_broadcast([128, 128]), [self.bcomb[j8]], [bcx])
                        self.mm(ps[:, jc * 128:(jc + 1) * 128], cx, self.ident(), True, True, [bcx, self.bcst], [bps], inc=True)
                    self.act(cb, ps[:], AF.Copy, [bps], [bcb])
                for c in range(nch):
                    psg, bpsg = self.next_ps()
                    for k in range(8):
                        wl, wbuf = wb_g.lhsT(k, c)
                        self.mm(psg[:], wl, self.h[:, t, k, :], k == 0, k == 7, [wbuf, self.bh[t]], [bpsg])
                    psu, bpsu = self.next_ps()
                    for k in range(8):
                        wl, wbuf = wb_u.lhsT(k, c)
                        self.mm(psu[:], wl, self.h[:, t, k, :], k == 0, k == 7, [wbuf, self.bh[t]], [bpsu])
                    sg, bsg = self.sg[:, c % 2, :], self.bsg[c % 2]
                    self.act(sg, psg[:], AF.Silu, [bpsg], [bsg])
                    if e is not None:
                        self.tt(DVE, sg, sg, self.cbc[:, t, :], ALU.mult, [bsg, self.bcbc[t]], [bsg])
                    self.tt(DVE, actv[:, c, :], psu[:], sg, ALU.mult, [bpsu, bsg], [bact])
                n = 0
                for (jd, svd, sbd) in sl_d:
                    for cc in range(dcols // 128):
                        ps, bps = self.next_ps()
                        for k in range(nch):
                            self.mm(ps[:], svd[:, k, cc * 128:(cc + 1) * 128], actv[:, k, :], k == 0, k == nch - 1,
                                    [sbd, bact], [bps])
                        self.tt(DVE, self.x[:, t, n, :], self.x[:, t, n, :], ps[:], ALU.add, [bps, self.bx[t][n]], [self.bx[t][n]])
                        n += 1
            wb_g.done()
            wb_u.done()
            for s in sl_d:
                R.done(s[0])
            g0 += G
            gi += 1

    def bcast_tok(self, dst, bdst, src_t, bsrc, e):
        for half in range(2):
            ps, bps = self.next_ps()
            for jc in range(4):
                c = half * 4 + jc
                cx, bcx = self.cexp[:, jc % 2, :], self.bcexp[jc % 2]
                self.cp(DVE, cx, src_t[:, c, e:e + 1].to_broadcast([128, 128]), [bsrc[c]], [bcx])
                self.mm(ps[:, jc * 128:(jc + 1) * 128], cx, self.ident(), True, True, [bcx, self.bcst], [bps], inc=True)
            self.act(dst[:, half * TT:(half + 1) * TT], ps[:], AF.Copy, [bps], [bdst])

    def phase_moe(self, l, p):
        P, R = self.P, self.R
        P.barrier(extra_bufs=[self.bofin])
        self.scratch("moe_a")
        self.bcomb = [Buf("comb%d" % j) for j in range(8)]
        self.bselt = [Buf("selt%d" % j) for j in range(8)]
        self.bselb = [Buf("selb%d" % j) for j in range(8)]
        self.bposm = [Buf("posm%d" % j) for j in range(8)]
        self.bcntf, self.bcnti = Buf("cntf"), Buf("cnti")
        bhT = [Buf("hT%d" % c) for c in range(8)]
        hT = self.mg[:].rearrange("p t k n -> p (t k n)").rearrange("p (c f) -> p c f", c=8)
        for t in range(NT):
            xi, bxi = self.xin_t(t)
            self.rmsnorm(xi, bxi, V_NFFN + 8 * l, None, None, out_f32=lambda kc: self.hf[:, kc, :], bout_f32=self.bhf)
            self.router(t)
            for jc in range(4):
                c = t * 4 + jc
                for g in range(2):
                    ps, bps = self.next_ps()
                    for k4 in range(4):
                        fch = g * 4 + k4
                        self.P.op(PE, lambda e, ps=ps, k4=k4, fch=fch, jc=jc: e.transpose(
                            ps[:, k4 * 128:(k4 + 1) * 128], self.hf[:, fch, jc * 128:(jc + 1) * 128], self.ident()),
                            [self.bhf, self.bcst], [bps], inc=(k4 == 3))
                    self.act(hT[:, c, g * 512:(g + 1) * 512], ps[:], AF.Copy, [bps], [bhT[c]])
        for c in range(8):
            ps, bps = self.next_ps()
            for c2 in range(c):
                self.mm(ps[:, 0:NE], self.ones_bf[:], self.selb_t[:, c2, :], c2 == 0, False, [self.bones, self.bselb[c2]], [bps])
            self.mm(ps[:, 0:NE], self.U_bf[:], self.selb_t[:, c, :], c == 0, True, [self.bU, self.bselb[c]], [bps])
            self.stt(DVE, self.posm_t[:, c, :], ps[:, 0:NE], 1.0, self.sel_t[:, c, :], ALU.add, ALU.mult,
                     [bps, self.bselt[c]], [self.bposm[c]])
            self.ts(DVE, self.posm_t[:, c, :], self.posm_t[:, c, :], -1.0, None, ALU.add, None, [self.bposm[c]], [self.bposm[c]])
        ps, bps = self.next_ps()
        for c in range(8):
            self.mm(ps[:, 0:NE], self.ones_bf[:], self.selb_t[:, c, :], c == 0, c == 7, [self.bones, self.bselb[c]], [bps])
        self.cp(DVE, self.cntf_t[:], ps[:, 0:NE], [bps], [self.bcntf])
        self.cp(DVE, self.cnti_t[:], self.cntf_t[0:1, :], [self.bcntf], [self.bcnti])
        P.barrier(extra_bufs=[self.bofin])
        self.scratch("moe_b")
        wgu_all, wdn_all = self.w_moe_gu[l // 2], self.w_moe_down[l // 2]
        ngrp = DFE // 512
        for e in range(NE):
            w_gu, w_down = wgu_all[e], wdn_all[e]
            P.cond_load(self.cnti_t[0:1, e:e + 1], self.bcnti)
            self.bcast_tok(self.combT, self.bcombT, self.comb_t, self.bcomb, e)
            self.bcast_tok(self.posT, self.bposT, self.posm_t, self.bposm, e)
            for gi in range(ngrp):
                g0 = gi * 512
                wb_g = WBlk(R, w_gu, g0, 512)
                wb_u = WBlk(R, w_gu, DFE + g0, 512)
                sl_d = [R.get(wsrc(w_down, g0, 512, c * 512, 512), 4) for c in range(2)]
                for i in range(MAXG):
                    P.cond_begin(None, None, i * GSL + 1)
                    he = self.he[:, 8 * i:8 * i + 8, :]
                    if gi == 0:
                        selv = self.selbuf.rearrange("p (c n) -> p c n", c=8)
                        for c in range(8):
                            self.ts(DVE, selv[:, c, :], self.iota_row(), self.posm_t[:, c, e:e + 1], float(-i * GSL),
                                    ALU.subtract, ALU.is_equal, [self.bcst, self.bposm[c]], [self.bsel])
                        for f in range(8):
                            ps, bps = self.next_ps()
                            for c in range(8):
                                self.mm(ps[:, 0:GSL], hT[:, c, f * 128:(f + 1) * 128], selv[:, c, :], c == 0, c == 7,
                                        [bhT[c], self.bsel], [bps])
                            self.act(he[:, f, :], ps[:, 0:GSL], AF.Copy, [bps], [self.bhe[i]])
                    a_i = (gi * MAXG + i) % 2
                    actv, bact = self.actb[:, 4 * a_i:4 * a_i + 4, :], self.bact[a_i]
                    for c4 in range(4):
                        psg, bpsg = self.next_ps()
                        for k in range(8):
                            wl, wbuf = wb_g.lhsT(k, c4)
                            self.mm(psg[:], wl, he[:, k, :], k == 0, k == 7, [wbuf, self.bhe[i]], [bpsg])
                        psu, bpsu = self.next_ps()
                        for k in range(8):
                            wl, wbuf = wb_u.lhsT(k, c4)
                            self.mm(psu[:], wl, he[:, k, :], k == 0, k == 7, [wbuf, self.bhe[i]], [bpsu])
                        sg, bsg = self.sg[:, c4 % 2, :], self.bsg[c4 % 2]
                        self.act(sg, psg[:], AF.Silu, [bpsg], [bsg])
                        self.tt(DVE, actv[:, c4, :], psu[:], sg, ALU.mult, [bpsu, bsg], [bact])
                    for q in range(NQ):
                        for half in range(2):
                            jd, svd, sbd = sl_d[half]
                            ps, bps = self.next_ps()
                            for k in range(4):
                                self.mm(ps[:], actv[:, k, q * 128:(q + 1) * 128], svd[:, k, :], k == 0, k == 3, [bact, sbd], [bps])
                            dst = self.yacc_parts[i][:, q, half * 512:(half + 1) * 512]
                            if gi == 0:
                                self.act(dst, ps[:], AF.Copy, [bps], [self.byacc[i][q]])
                            else:
                                self.tt(DVE, dst, dst, ps[:], ALU.add, [bps, self.byacc[i][q]], [self.byacc[i][q]])
                    if gi == ngrp - 1:
                        selT = self.selbuf.rearrange("p (q n) -> p q n", q=NQ)
                        for q in range(NQ):
                            self.ts(DVE, selT[:, q, :], self.posT, self.iota_col(), float(i * GSL + q * 128),
                                    ALU.subtract, ALU.is_equal, [self.bposT, self.bcst], [self.bsel])
                            self.act(self.yb[:, q, :], self.yacc_parts[i][:, q, :], AF.Copy, [self.byacc[i][q]], [self.byb[q]])
                        for t in range(NT):
                            for f in range(8):
                                ps, bps = self.next_ps()
                                for q in range(NQ):
                                    self.mm(ps[:], self.yb[:, q, f * 128:(f + 1) * 128], selT[:, q, t * TT:(t + 1) * TT], q == 0, q == NQ - 1,
                                            [self.byb[q], self.bsel], [bps])
                                tmp, btmp = self.stmp[:, f % 2, :], self.bstmp[f % 2]
                                self.tt(DVE, tmp, ps[:], self.combT[:, t * TT:(t + 1) * TT], ALU.mult, [bps, self.bcombT], [btmp])
                                self.tt(DVE, self.x[:, t, f, :], self.x[:, t, f, :], tmp, ALU.add, [btmp, self.bx[t][f]], [self.bx[t][f]])
                    if i % 2 == 1:
                        P.cond_end()
                        P.cond_end()
                wb_g.done()
                wb_u.done()
                for s_ in sl_d:
                    R.done(s_[0])

    def phase_ffn(self, l, p):
        if l % 2 == 1:
            return self.phase_moe(l, p)
        P = self.P
        P.barrier(extra_bufs=[self.bofin])
        self.scratch("ffn")
        moe = (l % 2 == 1)
        for t in range(NT):
            xi, bxi = self.xin_t(t)
            if moe:
                self.rmsnorm(xi, bxi, V_NFFN + 8 * l, lambda kc, t=t: self.h[:, t, kc, :], self.bh[t],
                             out_f32=lambda kc: self.hf[:, kc, :], bout_f32=self.bhf)
                self.router(t)
            else:
                self.rmsnorm(xi, bxi, V_NFFN + 8 * l, lambda kc, t=t: self.h[:, t, kc, :], self.bh[t])
        if moe:
            for e in range(NE):
                self.ffn_expert(self.w_moe_gu[l // 2, e], self.w_moe_down[l // 2, e], DFE, e=e)
        else:
            self.ffn_expert(self.w_ff_gu[l // 2], self.w_ff_down[l // 2], DFF)

    def dump_x(self, p, bout):
        P = self.P
        for t in range(NT):
            tok0 = p * TP + t * TT
            P.dma(SP, self.outT[:, tok0:tok0 + TT].rearrange("(kc p) n -> p kc n", p=128), self.x[:, t],
                  reads=[self.bx[t][k] for k in range(8)])

    def phase_final(self, p):
        P = self.P
        P.barrier(extra_bufs=[self.bofin])
        self.scratch("fin")
        for t in range(NT):
            xi, bxi = self.xin_t(t)
            self.rmsnorm(xi, bxi, V_NFIN, None, None, out_f32=lambda kc: self.ofin[:, kc, :], bout_f32=self.bofin)
            tok0 = p * TP + t * TT
            P.dma(SP, self.outT[:, tok0:tok0 + TT].rearrange("(kc p) n -> p kc n", p=128), self.ofin, reads=[self.bofin])
            if p + 1 < self.n_pass:
                tok1 = (p + 1) * TP + t * TT
                P.dma(POOL, self.x[:, t], self.xT[:, tok1:tok1 + TT].rearrange("(kc p) n -> p kc n", p=128),
                      writes=[self.bx[t][k] for k in range(8)])
            self.x_prefetched = True

    def body(self, P, R):
        self.P, self.R = P, R
        self.ps_i = 0
        self.bofin = Buf("ofin")
        self.load_consts()
        self.compute_memn()
        self.x_prefetched = False
        for p in range(self.n_pass):
            for t in range(NT):
                if p > 0 and self.x_prefetched:
                    break
                tok0 = p * TP + t * TT
                P.dma(SP, self.x[:, t], self.xT[:, tok0:tok0 + TT].rearrange("(kc p) n -> p kc n", p=128),
                      writes=[self.bx[t][k] for k in range(8)])
            stopped = False
            for l in range(DEPTH):
                self.load_layer_consts(l)
                self.phase_mixer(l, p)
                if self.stop_after in (("mixA", l), ("mixB", l), ("mix", l)):
                    stopped = True
                    break
                self.phase_xattn(l, p)
                if self.stop_after in (("xatO", l), ("xat", l)):
                    stopped = True
                    break
                self.phase_ffn(l, p)
                if self.stop_after == ("ffn", l):
                    stopped = True
                    break
            if stopped:
                self.dump_x(p, None)
            else:
                self.phase_final(p)
        outs = [self.bofin] + [self.bx[t][k] for t in range(NT) for k in range(8)]
        P.final_wait(SP, outs)


V_NMIX = 0
V_NXAT = 16
V_NFFN = 32
V_PSC = 48
V_NMEM = 56
V_NFIN = 64
NVEC = 72


def build_nc(n_pass=NPASS, stop_after=None):
    nc = bass.Bass("TRN2", target_bir_lowering=False)
    with ExitStack() as st:
        B = Builder(nc, st, n_pass=n_pass, stop_after=stop_after)
        dryP = Prog(nc, st, dry=True)
        dryR = Ring(dryP, B.ring_t, plan=None)
        B.body(dryP, dryR)
        plan = dryR.plan
        P = Prog(nc, st)
        cache = None
        if n_pass > 1:
            assert len(plan) % n_pass == 0
            cache = nc.dram_tensor("wcache", [len(plan) // n_pass, 128, SLAB], BF16).ap()
        R = Ring(P, B.ring_t, plan=plan, cache=cache, n_pass=n_pass)
        B.body(P, R)
        assert R.idx == len(plan)
        P.emit_all()
    return nc


def _cols(v):
    v = np.asarray(v, np.float32)
    return np.ascontiguousarray(v.reshape(-1, 128).T)


def host_consts():
    c = np.zeros((128, 960), np.float32)
    c[:, 0:128] = np.eye(128, dtype=np.float32)
    s = np.arange(128)[:, None]
    t = np.arange(128)[None, :]
    c[:, 128:256] = (t >= s).astype(np.float32)
    for g in range(4):
        w = 2 << g
        tt = np.arange(16)
        c[:, 256 + g * 16:256 + (g + 1) * 16] = (w / np.minimum(tt + 1, w)).astype(np.float32)[None, :]
    c[:, 320:832] = np.arange(512, dtype=np.float32)[None, :]
    c[:, 832] = np.arange(128, dtype=np.float32)
    return c


def prep_shared(inp):
    f = lambda a: np.ascontiguousarray(np.asarray(a, np.float32))
    vec = np.zeros((128, NVEC), np.float32)
    for l in range(DEPTH):
        vec[:, V_NMIX + 8 * l:V_NMIX + 8 * l + 8] = _cols(inp["norm_mix"][l])
        vec[:, V_NXAT + 8 * l:V_NXAT + 8 * l + 8] = _cols(inp["norm_xattn"][l])
        vec[:, V_NFFN + 8 * l:V_NFFN + 8 * l + 8] = _cols(inp["norm_ffn"][l])
        vec[:, V_PSC + 4 * l:V_PSC + 4 * l + 4] = _cols(inp["pool_scale"][l])
    vec[:, V_NMEM:V_NMEM + 8] = _cols(inp["norm_mem"])
    vec[:, V_NFIN:V_NFIN + 8] = _cols(inp["norm_final"])
    bcs = np.zeros((DEPTH, 128, 1536), np.float32)
    for l in range(DEPTH):
        bcs[l, :, 0:512] = np.asarray(inp["sgu_ln_g"][l], np.float32)[None, :]
        bcs[l, :, 512:1024] = np.asarray(inp["sgu_ln_b"][l], np.float32)[None, :]
        bcs[l, :, 1024:1536] = np.asarray(inp["b_spatial"][l], np.float32).reshape(1, 512)
    sh = {
        "vecs": vec,
        "bcs": bcs,
        "cst": host_consts(),
        "w_in": f(inp["w_in"]),
        "pool_mix": f(np.asarray(inp["pool_mix"]).transpose(0, 2, 1, 3)),
        "wspT": f(np.asarray(inp["w_spatial"]).transpose(0, 3, 1, 2)),
        "w_branch_a": f(inp["w_branch_a"]),
        "w_branch_b": f(inp["w_branch_b"]),
        "w_out": f(inp["w_out"]),
        "w_xq": f(inp["w_xq"]),
        "w_xkv": f(inp["w_xkv"]),
        "w_xo": f(inp["w_xo"]),
        "w_ff_gu": f(inp["w_ff_gu"]),
        "w_ff_down": f(inp["w_ff_down"]),
        "w_router": f(np.asarray(inp["w_router"])[0].reshape(8, 128, NE).transpose(1, 0, 2)),
        "w_moe_gu": f(inp["w_moe_gu"]),
        "w_moe_down": f(inp["w_moe_down"]),
    }
    return sh


_NC_CACHE = {}


def kernel(**inputs):
    x = np.asarray(inputs["x"], np.float32)
    mem = np.asarray(inputs["mem"], np.float32)
    nb = x.shape[0]
    sh = prep_shared(inputs)
    in_maps = []
    for b in range(nb):
        m = dict(sh)
        m["xT"] = np.ascontiguousarray(x[b].T)
        m["memT"] = np.ascontiguousarray(mem[b].T)
        in_maps.append(m)
    if "nc" not in _NC_CACHE:
        _NC_CACHE["nc"] = build_nc()
    res = run_bass_kernel_spmd(_NC_CACHE["nc"], in_maps, core_ids=list(range(nb)))
    out = np.stack([np.ascontiguousarray(r["outT"].T) for r in res.results], axis=0)
    return out.astype(np.float32)
```

```python
from contextlib import ExitStack
import numpy as np
import concourse.bass as bass
import concourse.mybir as mybir
from concourse.bass_utils import run_bass_kernel_spmd

F32 = mybir.dt.float32
I32 = mybir.dt.int32
BF16 = mybir.dt.bfloat16
AF = mybir.ActivationFunctionType
ALU = mybir.AluOpType
AX = mybir.AxisListType

PE, ACT, DVE, POOL, SP = "pe", "act", "dve", "pool", "sp"
COMPUTE = (PE, ACT, DVE, POOL)

D = 1024
SEQ = 4096
MEM = 256
DEPTH = 2
TT = 512
NT = 2
TP = TT * NT
NPASS = SEQ // TP
DFF = 2816
NE = 8
DFE = 3584
EPS = 1e-6
NS = 12
SLAB = 2048
GSL = 512
NQ = GSL // 128
MAXG = TP // GSL
NSTAGE = 2
SCRF = 8448
SCRB = 20480


class Buf:
    __slots__ = ("name", "last_w", "readers", "dma_sem", "dma_sem_hw")

    def __init__(self, name):
        self.name = name
        self.last_w = None
        self.readers = []
        self.dma_sem = None
        self.dma_sem_hw = None


class Prog:
    def __init__(self, nc, stack, dry=False):
        self.nc = nc
        self.stack = stack
        self.dry = dry
        self.ops = {e: [] for e in (PE, ACT, DVE, POOL, SP)}
        self.sems = {}
        self.cnt = {}
        self.seen = {e: {} for e in self.ops}
        self.n_dma_sems = 0
        self.cstack = []
        if not dry:
            for e in COMPUTE:
                self._mksem(e)
            self.regs = {PE: stack.enter_context(nc.tensor.register("r_pe")),
                         ACT: stack.enter_context(nc.scalar.register("r_act")),
                         DVE: stack.enter_context(nc.vector.register("r_dve"))}

    def _mksem(self, key):
        self.sems[key] = self.stack.enter_context(self.nc.semaphore("s_" + key))
        self.cnt[key] = 0

    def _waits_for(self, eng, reads, writes, extra=()):
        need = {}

        def req(dep):
            if dep is None:
                return
            k, v = dep
            if need.get(k, 0) < v:
                need[k] = v
        for b in reads:
            req(b.last_w)
        for b in writes:
            req(b.last_w)
            for r in b.readers:
                req(r)
        for d in extra:
            req(d)
        out = []
        seen = self.seen[eng]
        for k, v in need.items():
            if k == PE and eng == PE:
                continue
            if seen.get(k, 0) < v:
                seen[k] = v
                out.append((k, v))
        return out

    def op(self, eng, emit, reads=(), writes=(), inc=True):
        if self.dry:
            return
        waits = self._waits_for(eng, reads, writes)
        if inc:
            self.cnt[eng] += 1
            me = (eng, self.cnt[eng])
            self.ops[eng].append(("op", waits, emit, eng, 1))
        else:
            me = (eng, self.cnt[eng] + 1)
            self.ops[eng].append(("op", waits, emit, None, 0))
        for b in reads:
            b.readers.append(me)
        for b in writes:
            b.last_w = me
            b.readers = []

    def dma(self, queue, out_ap, in_ap, reads=(), writes=(), extra=()):
        if self.dry:
            return None
        owner = (list(writes) + list(reads))[0]
        attr = "dma_sem" if queue == POOL else "dma_sem_hw"
        if getattr(owner, attr) is None:
            setattr(owner, attr, "d%d" % self.n_dma_sems)
            self.n_dma_sems += 1
            self._mksem(getattr(owner, attr))
        semkey = getattr(owner, attr)
        waits = self._waits_for(queue, reads, writes, extra=extra)
        self.cnt[semkey] += 16
        me = (semkey, self.cnt[semkey])
        self.ops[queue].append(("op", waits, lambda e: e.dma_start(out=out_ap, in_=in_ap), semkey, 16))
        for b in reads:
            b.readers.append(me)
        for b in writes:
            b.last_w = me
            b.readers = []
        return me

    def barrier(self, extra_bufs=()):
        if self.dry:
            return
        deps = [(e, self.cnt[e]) for e in COMPUTE if self.cnt[e] > 0]
        for e in COMPUTE:
            waits = self._waits_for(e, extra_bufs, extra_bufs, extra=deps)
            if waits:
                self.ops[e].append(("op", waits, None, None, 0))

    def cond_load(self, cnt_ap, cnt_buf):
        if self.dry:
            return
        for e in (PE, ACT, DVE):
            waits = self._waits_for(e, [cnt_buf], [])
            cnt_buf.readers.append((e, self.cnt[e] + 1))
            self.ops[e].append(("cload", waits, cnt_ap))

    def cond_begin(self, cnt_ap, cnt_buf, thresh):
        if self.dry:
            return
        info = {"ap": cnt_ap, "thresh": thresh, "n": {}, "start": {}, "snap": {}, "pos": {}}
        for e in (PE, ACT, DVE):
            waits = []
            if cnt_ap is not None:
                waits = self._waits_for(e, [cnt_buf], [])
                cnt_buf.readers.append((e, self.cnt[e] + 1))
            info["start"][e] = self.cnt[e]
            info["snap"][e] = dict(self.seen[e])
            info["pos"][e] = len(self.ops[e])
            self.ops[e].append(("cbegin", waits, info))
        self.cstack.append(info)

    def cond_end(self):
        if self.dry:
            return
        info = self.cstack.pop()
        for e in (PE, ACT, DVE):
            info["n"][e] = self.cnt[e] - info["start"][e]
            assert info["n"][e] > 0, "conditional block without incrementing op on " + e
            self.seen[e] = info["snap"][e]
            self.ops[e].append(("cend", info))

    def final_wait(self, eng, bufs):
        if self.dry:
            return
        waits = self._waits_for(eng, bufs, bufs)
        self.ops[eng].append(("op", waits, None, None, 0))

    def emit_all(self):
        nc = self.nc
        sems = self.sems
        with nc.Block() as block:
            def run(eng_name):
                def body(e):
                    gstack = []
                    for ent in self.ops[eng_name]:
                        kind = ent[0]
                        if kind == "op":
                            _, waits, emit, semkey, inc = ent
                            for k, v in waits:
                                e.wait_ge(sems[k], v)
                            if emit is not None:
                                ins = emit(e)
                                if inc:
                                    ins.then_inc(sems[semkey], inc)
                        elif kind == "cload":
                            _, waits, cap = ent
                            for k, v in waits:
                                e.wait_ge(sems[k], v)
                            e.reg_load(self.regs[eng_name], cap)
                        elif kind == "cbegin":
                            _, waits, info = ent
                            for k, v in waits:
                                e.wait_ge(sems[k], v)
                            reg = self.regs[eng_name]
                            if info["ap"] is not None:
                                e.reg_load(reg, info["ap"])
                            g = e.If_lt(reg, info["thresh"])
                            g.__enter__()
                            e.drain()
                            e.sem_inc(sems[eng_name], info["n"][eng_name])
                            g.__exit__(None, None, None)
                            g2 = e.Else()
                            g2.__enter__()
                            gstack.append(g2)
                        else:
                            gstack.pop().__exit__(None, None, None)
                return body
            block.tensor(run(PE))
            block.scalar(run(ACT))
            block.vector(run(DVE))
            block.gpsimd(run(POOL))
            block.sync(run(SP))


class Ring:
    def __init__(self, P, ring_t, plan=None, cache=None, n_pass=1):
        self.P = P
        self.ring_t = ring_t
        self.cache = cache
        self.n_pass = n_pass
        self.cache_ready = {}
        self.dry = plan is None
        self.plan = [] if plan is None else plan
        self.idx = 0
        self.next_load = 0
        self.released = [False] * (len(self.plan) if plan else 0)
        self.bufs = [Buf("slab%d" % i) for i in range(NS)]

    def view(self, slot, kc):
        return self.ring_t[:, slot, :].rearrange("p (kc n) -> p kc n", kc=kc)

    def _pump(self):
        while self.next_load < len(self.plan):
            j = self.next_load
            if j >= NS and not self.released[j - NS]:
                break
            src, kc = self.plan[j]
            slot = j % NS
            npp = len(self.plan) // self.n_pass
            if self.cache is None or self.n_pass == 1:
                self.P.dma(POOL, self.view(slot, kc), src, writes=[self.bufs[slot]])
            elif j < npp:
                self.P.dma(POOL, self.view(slot, kc), src, writes=[self.bufs[slot]])
                cview = self.cache[j].rearrange("p (kc n) -> p kc n", kc=kc)
                self.cache_ready[j] = self.P.dma(SP, cview, self.view(slot, kc), reads=[self.bufs[slot]])
            else:
                jj = j % npp
                assert self.plan[jj][1] == kc
                cview = self.cache[jj].rearrange("p (kc n) -> p kc n", kc=kc)
                self.P.dma(SP, self.view(slot, kc), cview, writes=[self.bufs[slot]], extra=[self.cache_ready[jj]])
            self.next_load += 1

    def get(self, src, kc):
        j = self.idx
        self.idx += 1
        if self.dry:
            self.plan.append((src, kc))
            return j, self.view(0, kc), self.bufs[0]
        self._pump()
        assert self.next_load > j, "ring too small for simultaneous residency (load %d)" % j
        slot = j % NS
        return j, self.view(slot, kc), self.bufs[slot]

    def done(self, j):
        if self.dry:
            return
        self.released[j] = True
        self._pump()


class WBlk:
    def __init__(self, R, w_ap, c0, ncols):
        self.R = R
        self.ncols = ncols
        if ncols == 512:
            self.sl = [R.get(wsrc(w_ap, h * 512, 512, c0, 512), 4) for h in range(2)]
        else:
            assert ncols == 256
            self.sl = [R.get(wsrc(w_ap, 0, D, c0, 256), 8)]

    def lhsT(self, k, c):
        if self.ncols == 512:
            s = self.sl[k // 4]
            return s[1][:, k % 4, c * 128:(c + 1) * 128], s[2]
        s = self.sl[0]
        return s[1][:, k, c * 128:(c + 1) * 128], s[2]

    def rows(self, k, c0, n):
        if self.ncols == 512:
            s = self.sl[k // 4]
            return s[1][:, k % 4, c0:c0 + n], s[2]
        s = self.sl[0]
        return s[1][:, k, c0:c0 + n], s[2]

    def done(self):
        for s in self.sl:
            self.R.done(s[0])


def wsrc(w_ap, r0, nr, c0, ncol):
    return w_ap[r0:r0 + nr, c0:c0 + ncol].rearrange("(kc p) n -> p kc n", p=128)


class Builder:
    def __init__(self, nc, st, n_pass=NPASS, stop_after=None):
        self.nc = nc
        self.st = st
        self.n_pass = n_pass
        self.stop_after = stop_after
        self.declare_dram()
        self.alloc()

    def declare_dram(self):
        nc = self.nc

        def inp(name, shape):
            return nc.dram_tensor(name, list(shape), F32, kind="ExternalInput").ap()
        self.xT = inp("xT", [D, SEQ])
        self.memT = inp("memT", [D, MEM])
        self.vecs = inp("vecs", [128, NVEC])
        self.bcs = inp("bcs", [DEPTH, 128, 1536])
        self.cst = inp("cst", [128, 960])
        self.w_in = inp("w_in", [DEPTH, D, 3584])
        self.pool_mix = inp("pool_mix", [DEPTH, 128, 4, 128])
        self.wspT = inp("wspT", [DEPTH, 128, 4, 128])
        self.w_bra = inp("w_branch_a", [DEPTH, 512, D])
        self.w_brb = inp("w_branch_b", [DEPTH, 512, D])
        self.w_out = inp("w_out", [DEPTH, D, D])
        self.w_xq = inp("w_xq", [DEPTH, D, D])
        self.w_xkv = inp("w_xkv", [DEPTH, D, 2 * D])
        self.w_xo = inp("w_xo", [DEPTH, D, D])
        self.w_ff_gu = inp("w_ff_gu", [1, D, 2 * DFF])
        self.w_ff_down = inp("w_ff_down", [1, DFF, D])
        self.w_router = inp("w_router", [128, 8, NE])
        self.w_moe_gu = inp("w_moe_gu", [1, NE, D, 2 * DFE])
        self.w_moe_down = inp("w_moe_down", [1, NE, DFE, D])
        self.outT = nc.dram_tensor("outT", [D, SEQ], F32, kind="ExternalOutput").ap()

    def sb(self, name, shape, dt):
        return self.st.enter_context(self.nc.sbuf_tensor(name, list(shape), dt))

    def alloc(self):
        nc = self.nc
        self.x = self.sb("x", [128, NT, 8, TT], F32)
        self.bx = [[Buf("x%d_%d" % (t, k)) for k in range(8)] for t in range(NT)]
        self.h = self.sb("h", [128, NT, 8, TT], BF16)
        self.bh = [Buf("h%d" % t) for t in range(NT)]
        self.mg = self.sb("mg", [128, NT, 8, TT], BF16)
        self.bmg = [[Buf("mg%d_%d" % (t, k)) for k in range(8)] for t in range(NT)]
        self.memn = self.sb("memn", [128, 8, MEM], BF16)
        self.bmemn = Buf("memn")
        self.ring_t = self.sb("ring", [128, NS, SLAB], BF16)
        self.vec_t = self.sb("vec_t", [128, NVEC], F32)
        self.bvec = Buf("vec")
        self.bc_t = self.sb("bc_t", [128, 1536], F32)
        self.bbc = Buf("bc")
        self.cst_t = self.sb("cst_t", [128, 960], F32)
        self.bcst = Buf("cst")
        self.ones_bf = self.sb("ones_bf", [128, 128], BF16)
        self.bones = Buf("ones")
        self.pmix = self.sb("pmix", [128, 4, 128], BF16)
        self.bpmix = Buf("pmix")
        self.wsp_f = self.sb("wsp_f", [128, 4, 128], F32)
        self.bwspf = Buf("wspf")
        self.wsp = self.sb("wsp", [128, 4, 128], BF16)
        self.bwsp = Buf("wsp")
        self.wr = self.sb("wr", [128, 8, NE], F32)
        self.bwr = Buf("wr")
        self.halo = self.sb("halo", [128, DEPTH, 4, 16], F32)
        self.bhalo = [Buf("halo%d" % l) for l in range(DEPTH)]
        self.bmemf = Buf("memf")
        self.U_bf = self.sb("U_bf", [128, 128], BF16)
        self.bU = Buf("U")
        self.comb_t = self.sb("comb_t", [128, 8, NE], F32)
        self.sel_t = self.sb("sel_t", [128, 8, NE], F32)
        self.selb_t = self.sb("selb_t", [128, 8, NE], BF16)
        self.posm_t = self.sb("posm_t", [128, 8, NE], F32)
        self.cntf_t = self.sb("cntf_t", [128, NE], F32)
        self.cnti_t = self.sb("cnti_t", [1, NE], I32)
        self.scr_f = self.sb("scr_f", [128, SCRF], F32)
        self.scr_b = self.sb("scr_b", [128, SCRB], BF16)
        self.psum = [self.st.enter_context(nc.psum_tensor("ps%d" % i, [128, 512], F32)) for i in range(8)]
        self.bps = [Buf("ps%d" % i) for i in range(8)]
        self.ps_i = 0

    def next_ps(self):
        i = self.ps_i % 8
        self.ps_i += 1
        return self.psum[i], self.bps[i]

    def mm(self, out, lhsT, rhs, start, stop, reads, writes, inc=None):
        self.P.op(PE, lambda e: e.matmul(out, lhsT=lhsT, rhs=rhs, start=start, stop=stop), reads, writes,
                  inc=(stop if inc is None else inc))

    def act(self, out, in_, func, reads, writes, **kw):
        self.P.op(ACT, lambda e: e.activation(out=out, in_=in_, func=func, **kw), reads, writes)

    def tt(self, eng, out, in0, in1, op, reads, writes):
        self.P.op(eng, lambda e: e.tensor_tensor(out=out, in0=in0, in1=in1, op=op), reads, writes)

    def ts(self, eng, out, in0, s1, s2, op0, op1, reads, writes):
        if op1 is None:
            s2, op1 = 0.0, ALU.add
        self.P.op(eng, lambda e: e.tensor_scalar(out=out, in0=in0, scalar1=s1, scalar2=s2, op0=op0, op1=op1), reads, writes)

    def stt(self, eng, out, in0, scalar, in1, op0, op1, reads, writes):
        self.P.op(eng, lambda e: e.scalar_tensor_tensor(out=out, in0=in0, scalar=scalar, in1=in1, op0=op0, op1=op1), reads, writes)

    def cp(self, eng, out, in_, reads, writes):
        self.P.op(eng, lambda e: e.tensor_copy(out=out, in_=in_), reads, writes)

    def scratch(self, phase):
        f, b = self.scr_f, self.scr_b
        s = {}

        def fv(name, off, shape):
            n = int(np.prod(shape))
            ap = f[:, off:off + n]
            if len(shape) == 2:
                ap = ap.rearrange("p (a b) -> p a b", a=shape[0])
            return ap, off + n

        def bv(name, off, shape):
            n = int(np.prod(shape))
            ap = b[:, off:off + n]
            if len(shape) == 2:
                ap = ap.rearrange("p (a b) -> p a b", a=shape[0])
            return ap, off + n
        of = 0
        ob = 0
        self.rstd_a, of = fv("rstd_a", of, [TT])
        self.rstd, of = fv("rstd", of, [TT])
        self.sq, ob = bv("sq", ob, [8, TT])
        self.brstd_a, self.brstd, self.bsq = Buf("rstd_a"), Buf("rstd"), Buf("sq")
        self.bsqk = [Buf("sq%d" % k) for k in range(8)]
        if phase == "base":
            self.memf, of = fv("memf", of, [8, MEM])
        elif phase == "mix":
            self.pext, of = fv("pext", of, [4, 528])
            self.bpext = [Buf("pext%d" % c) for c in range(4)]
            self.pltmp, of = fv("pltmp", of, [2, 528])
            self.bpltmp = [Buf("pltmp%d" % i) for i in range(2)]
            self.vtm, of = fv("vtm", of, [4, TT])
            self.bvtm = [Buf("vtm%d" % i) for i in range(4)]
            self.tmpf, of = fv("tmpf", of, [2, TT])
            self.btmpf = [Buf("tmpf%d" % i) for i in range(2)]
            self.stat, of = fv("stat", of, [4, 16])
            self.bstat = [Buf("stat%d" % i) for i in range(4)]
            self.pooled, ob = bv("pooled", ob, [4, TT])
            self.bpooled = [Buf("pooled%d" % c) for c in range(4)]
            self.mixed, ob = bv("mixed", ob, [4, TT])
            self.bmixed = Buf("mixed")
            self.sig, ob = bv("sig", ob, [8, TT])
            self.bsig = [Buf("sig%d" % i) for i in range(8)]
            self.gu, ob = bv("gu", ob, [4, TT])
            self.bgu = [Buf("gu%d" % c) for c in range(4)]
            self.vn, ob = bv("vn", ob, [4, TT])
            self.bvn = [Buf("vn%d" % j) for j in range(4)]
            self.sgu, ob = bv("sgu", ob, [4, TT])
            self.bsgu = Buf("sgu")
        elif phase == "xat":
            self.rs, of = fv("rs", of, [2, TT])
            self.brs = [Buf("rs%d" % i) for i in range(2)]
            self.KT, ob = bv("KT", ob, [8, MEM])
            self.bKT = Buf("KT")
            self.V, ob = bv("V", ob, [2, D])
            self.bV = Buf("V")
            self.eT, ob = bv("eT", ob, [4, TT])
            self.beT = [Buf("eT%d" % i) for i in range(2)]
        elif phase == "ffn":
            self.hf, of = fv("hf", of, [8, TT])
            self.bhf = Buf("hf")
            self.sg, of = fv("sg", of, [2, TT])
            self.bsg = [Buf("sg%d" % i) for i in range(2)]
            self.cbc, of = fv("cbc", of, [2, TT])
            self.bcbc = [Buf("cbc%d" % i) for i in range(2)]
            self.cexp, of = fv("cexp", of, [2, 128])
            self.bcexp = [Buf("cexp%d" % i) for i in range(2)]
            self.comb, of = fv("comb", of, [8, NE])
            self.bcomb = [Buf("comb%d" % j) for j in range(8)]
            self.rt, of = fv("rt", of, [8, 16])
            self.brt = [Buf("rt%d" % i) for i in range(2)]
            self.actb, ob = bv("actb", ob, [8, TT])
            self.bact = [Buf("act%d" % i) for i in range(2)]
        elif phase == "moe_a":
            self.hf, of = fv("hf", of, [8, TT])
            self.bhf = Buf("hf")
            self.rt, of = fv("rt", of, [8, 16])
            self.brt = [Buf("rt%d" % i) for i in range(2)]
        elif phase == "moe_b":
            of = 0
            ob = 0
            yacc_lo, of = fv("yacc", of, [NQ, D])
            yacc_hi = self.h[:].rearrange("p t k n -> p (t k n)").bitcast(F32).rearrange("p (a b) -> p a b", a=NQ)
            self.yacc_parts = [yacc_lo, yacc_hi]
            assert MAXG == 2 and NQ == 4
            self.byacc = [[Buf("yacc%d_%d" % (i, q)) for q in range(NQ)] for i in range(MAXG)]
            self.combT, of = fv("combT", of, [TP])
            self.bcombT = Buf("combT")
            self.posT, of = fv("posT", of, [TP])
            self.bposT = Buf("posT")
            self.sg, of = fv("sg", of, [2, GSL])
            self.bsg = [Buf("sg%d" % i) for i in range(2)]
            self.cexp, of = fv("cexp", of, [2, 128])
            self.bcexp = [Buf("cexp%d" % i) for i in range(2)]
            self.stmp, of = fv("stmp", of, [2, TT])
            self.bstmp = [Buf("stmp%d" % i) for i in range(2)]
            self.actb, ob = bv("actb", ob, [8, GSL])
            self.bact = [Buf("act%d" % i) for i in range(2)]
            self.selbuf, ob = bv("selbuf", ob, [8 * GSL])
            self.bsel = Buf("selbuf")
            self.yb, ob = bv("yb", ob, [NQ, D])
            self.byb = [Buf("yb%d" % q) for q in range(NQ)]
            self.he, ob = bv("he", ob, [MAXG * 8, GSL])
            self.bhe = [[Buf("he%d_%d" % (i, f)) for f in range(8)] for i in range(MAXG)]
        elif phase == "fin":
            self.ofin, of = fv("ofin", of, [8, TT])
        assert of <= SCRF and ob <= SCRB, (phase, of, ob)

    def vcol(self, c0, n=1):
        return self.vec_t[:, c0:c0 + n]

    def rmsnorm(self, xin, bxin, gcol, out_bf, bout, n=TT, out_f32=None, bout_f32=None):
        for kc in range(8):
            if kc % 2 == 0:
                self.act(self.sq[:, kc, 0:n], xin(kc), AF.Square, [bxin(kc)], [self.bsqk[kc]])
            else:
                self.tt(DVE, self.sq[:, kc, 0:n], xin(kc), xin(kc), ALU.mult, [bxin(kc)], [self.bsqk[kc]])
        ps, bps = self.next_ps()
        for kc in range(8):
            self.mm(ps[:, 0:n], self.ones_bf[:], self.sq[:, kc, 0:n], kc == 0, kc == 7, [self.bones, self.bsqk[kc]], [bps])
        self.act(self.rstd_a[:, 0:n], ps[:, 0:n], AF.Sqrt, [bps], [self.brstd_a], bias=EPS, scale=1.0 / D)
        self.P.op(DVE, lambda e: e.reciprocal(out=self.rstd[:, 0:n], in_=self.rstd_a[:, 0:n]), [self.brstd_a], [self.brstd])
        for kc in range(8):
            if out_bf is not None:
                self.stt(DVE, out_bf(kc), xin(kc), self.vcol(gcol + kc), self.rstd[:, 0:n], ALU.mult, ALU.mult,
                         [bxin(kc), self.bvec, self.brstd], [bout])
            if out_f32 is not None:
                self.stt(DVE, out_f32(kc), xin(kc), self.vcol(gcol + kc), self.rstd[:, 0:n], ALU.mult, ALU.mult,
                         [bxin(kc), self.bvec, self.brstd], [bout_f32])

    def xin_t(self, t):
        return (lambda kc: self.x[:, t, kc, :]), (lambda kc: self.bx[t][kc])

    def proj_accum_x(self, t, blks, src_chunks, bsrc, kchunks):
        n = 0
        for wb in blks:
            for c in range(wb.ncols // 128):
                ps, bps = self.next_ps()
                for k in range(kchunks):
                    wl, wbuf = wb.lhsT(k, c)
                    self.mm(ps[:], wl, src_chunks(k), k == 0, k == kchunks - 1, [wbuf] + bsrc, [bps])
                self.tt(DVE, self.x[:, t, n, :], self.x[:, t, n, :], ps[:], ALU.add, [bps, self.bx[t][n]], [self.bx[t][n]])
                n += 1

    def load_consts(self):
        P = self.P
        P.dma(SP, self.vec_t[:], self.vecs, writes=[self.bvec])
        P.dma(SP, self.cst_t[:], self.cst, writes=[self.bcst])
        P.dma(SP, self.wr[:], self.w_router, writes=[self.bwr])
        P.op(DVE, lambda e: e.memset(self.ones_bf[:], 1.0), [], [self.bones])
        P.op(DVE, lambda e: e.memset(self.halo[:], 0.0), [], self.bhalo)
        self.tt(DVE, self.U_bf[:], self.mask(), self.ident(), ALU.subtract, [self.bcst], [self.bU])

    def ident(self):
        return self.cst_t[:, 0:128]

    def mask(self):
        return self.cst_t[:, 128:256]

    def iota_row(self):
        return self.cst_t[:, 320:320 + GSL]

    def iota_col(self):
        return self.cst_t[:, 832:833]

    def ratio(self, c):
        return self.cst_t[:, 256 + c * 16:256 + (c + 1) * 16]

    def compute_memn(self):
        self.scratch("base")
        self.P.dma(SP, self.memf, self.memT.rearrange("(kc p) m -> p kc m", p=128), writes=[self.bmemf])
        self.rmsnorm(lambda kc: self.memf[:, kc, :], lambda kc: self.bmemf, V_NMEM,
                     lambda kc: self.memn[:, kc, :], self.bmemn, n=MEM)

    def load_layer_consts(self, l):
        P = self.P
        P.dma(SP, self.bc_t[:], self.bcs[l], writes=[self.bbc])
        P.dma(POOL, self.pmix[:], self.pool_mix[l], writes=[self.bpmix])
        P.dma(SP, self.wsp_f[:], self.wspT[l], writes=[self.bwspf])
        for g in range(4):
            self.tt(DVE, self.wsp[:, g, :], self.wsp_f[:, g, :], self.mask(), ALU.mult, [self.bwspf, self.bcst], [self.bwsp])

    def phase_mixer(self, l, p):
        P, R = self.P, self.R
        P.barrier(extra_bufs=[self.bofin])
        self.scratch("mix")
        win = self.w_in[l]
        for t in range(NT):
            xi, bxi = self.xin_t(t)
            self.rmsnorm(xi, bxi, V_NMIX + 8 * l, lambda kc, t=t: self.h[:, t, kc, :], self.bh[t])
        wb_p = WBlk(R, win, 0, 512)
        wb_ga = [WBlk(R, win, 1536 + c * 512, 512) for c in range(2)]
        sl_bra = [R.get(wsrc(self.w_bra[l], 0, 512, c * 512, 512), 4) for c in range(2)]
        for t in range(NT):
            gt = p * NT + t
            hk = lambda k, t=t: self.h[:, t, k, :]
            for c in range(4):
                ps, bps = self.next_ps()
                for k in range(8):
                    wl, wbuf = wb_p.lhsT(k, c)
                    self.mm(ps[:], wl, hk(k), k == 0, k == 7, [wbuf, self.bh[t]], [bps])
                self.act(self.pext[:, c, 16:528], ps[:], AF.Copy, [bps], [self.bpext[c]])
                if gt == 0:
                    self.P.op(DVE, lambda e, c=c: e.memset(self.pext[:, c, 0:16], 0.0), [], [self.bpext[c]])
                else:
                    self.cp(DVE, self.pext[:, c, 0:16], self.halo[:, l, c, :], [self.bhalo[l]], [self.bpext[c]])
            for c in range(4):
                self.cp(DVE, self.halo[:, l, c, :], self.pext[:, c, 512:528], [self.bpext[c]], [self.bhalo[l]])
            for n in range(8):
                ps, bps = self.next_ps()
                for k in range(8):
                    wl, wbuf = wb_ga[n // 4].lhsT(k, n % 4)
                    self.mm(ps[:], wl, hk(k), k == 0, k == 7, [wbuf, self.bh[t]], [bps])
                self.act(self.sig[:, n, :], ps[:], AF.Sigmoid, [bps], [self.bsig[n]])
            for c in range(4):
                w = 2 << c
                cur, bcur = self.pext[:, c, :], self.bpext[c]
                for k in range(c + 1):
                    sh = 1 << k
                    lo = (2 << k) - 1
                    nxt, bnxt = self.pltmp[:, k % 2, :], self.bpltmp[k % 2]
                    self.tt(DVE, nxt[:, lo:528], cur[:, lo:528], cur[:, lo - sh:528 - sh], ALU.add, [bcur], [bnxt])
                    cur, bcur = nxt, bnxt
                if gt == 0:
                    self.tt(DVE, cur[:, 16:32], cur[:, 16:32], self.ratio(c), ALU.mult, [bcur, self.bcst], [bcur])
                self.stt(DVE, self.pooled[:, c, :], cur[:, 16:528], 1.0 / w, self.pext[:, c, 16:528], ALU.mult, ALU.subtract,
                         [bcur, self.bpext[c]], [self.bpooled[c]])
            for c in range(4):
                ps, bps = self.next_ps()
                self.mm(ps[:], self.pmix[:, c, :], self.pooled[:, c, :], True, True, [self.bpmix, self.bpooled[c]], [bps])
                self.act(self.mixed[:, c, :], ps[:], AF.Identity, [bps, self.bvec], [self.bmixed], scale=self.vcol(V_PSC + 4 * l + c))
            for n in range(8):
                j2, sv2, sbuf2 = sl_bra[n // 4]
                ps2, bps2 = self.next_ps()
                for k in range(4):
                    self.mm(ps2[:], sv2[:, k, (n % 4) * 128:(n % 4 + 1) * 128], self.mixed[:, k, :], k == 0, k == 3,
                            [sbuf2, self.bmixed], [bps2])
                self.tt(DVE, self.mg[:, t, n, :], ps2[:], self.sig[:, n, :], ALU.mult, [bps2, self.bsig[n]], [self.bmg[t][n]])
        for wb in [wb_p] + wb_ga:
            wb.done()
        for s in sl_bra:
            R.done(s[0])
        if self.stop_after == ("mixA", l):
            return
        wb_u = WBlk(R, win, 512, 512)
        wb_v = WBlk(R, win, 1024, 512)
        wb_gb = [WBlk(R, win, 2560 + c * 512, 512) for c in range(2)]
        sl_brb = [R.get(wsrc(self.w_brb[l], 0, 512, c * 512, 512), 4) for c in range(2)]
        for t in range(NT):
            hk = lambda k, t=t: self.h[:, t, k, :]
            for jc in range(4):
                vt, bvt = self.vtm[:, jc, :], self.bvtm[jc]
                for half in range(2):
                    ps, bps = self.next_ps()
                    for k in range(8):
                        wr_, wbuf = wb_v.rows(k, half * 256, 256)
                        self.mm(ps[:, 0:256], self.h[:, t, k, jc * 128:(jc + 1) * 128], wr_, k == 0, k == 7,
                                [wbuf, self.bh[t]], [bps])
                    self.act(vt[:, half * 256:(half + 1) * 256], ps[:, 0:256], AF.Gelu_apprx_tanh, [bps], [bvt])
            for c in range(4):
                ps, bps = self.next_ps()
                for k in range(8):
                    wl, wbuf = wb_u.lhsT(k, c)
                    self.mm(ps[:], wl, hk(k), k == 0, k == 7, [wbuf, self.bh[t]], [bps])
                self.act(self.gu[:, c, :], ps[:], AF.Gelu_apprx_tanh, [bps], [self.bgu[c]])
            for jc in range(4):
                vt, bvt = self.vtm[:, jc, :], self.bvtm[jc]
                st_, bst = self.stat[:, jc, :], self.bstat[jc]
                self.P.op(DVE, lambda e, vt=vt, st_=st_: e.bn_stats(out=st_[:, 0:6], in_=vt), [bvt], [bst])
                self.P.op(DVE, lambda e, st_=st_: e.bn_aggr(out=st_[:, 6:8], in_=st_[:, 0:6]), [bst], [bst])
                self.act(st_[:, 8:9], st_[:, 7:8], AF.Sqrt, [bst], [bst], bias=EPS, scale=1.0)
                self.P.op(DVE, lambda e, st_=st_: e.reciprocal(out=st_[:, 9:10], in_=st_[:, 8:9]), [bst], [bst])
                self.ts(DVE, vt, vt, st_[:, 6:7], st_[:, 9:10], ALU.subtract, ALU.mult, [bvt, bst], [bvt])
                self.tt(DVE, vt, vt, self.bc_t[:, 0:512], ALU.mult, [bvt, self.bbc], [bvt])
                self.tt(DVE, self.vn[:, jc, :], vt, self.bc_t[:, 512:1024], ALU.add, [bvt, self.bbc], [self.bvn[jc]])
            for n in range(8):
                ps, bps = self.next_ps()
                for k in range(8):
                    wl, wbuf = wb_gb[n // 4].lhsT(k, n % 4)
                    self.mm(ps[:], wl, hk(k), k == 0, k == 7, [wbuf, self.bh[t]], [bps])
                self.act(self.sig[:, n, :], ps[:], AF.Sigmoid, [bps], [self.bsig[n]])
            for g in range(4):
                ps, bps = self.next_ps()
                for jc in range(4):
                    self.mm(ps[:, jc * 128:(jc + 1) * 128], self.vn[:, jc, g * 128:(g + 1) * 128], self.wsp[:, g, :], True, True,
                            [self.bvn[jc], self.bwsp], [bps], inc=(jc == 3))
                tf, btf = self.tmpf[:, g % 2, :], self.btmpf[g % 2]
                for jc in range(4):
                    self.tt(DVE, tf[:, jc * 128:(jc + 1) * 128], ps[:, jc * 128:(jc + 1) * 128],
                            self.bc_t[:, 1024 + g * 128:1024 + (g + 1) * 128], ALU.add, [bps, self.bbc], [btf])
                self.tt(DVE, self.sgu[:, g, :], tf, self.gu[:, g, :], ALU.mult, [btf, self.bgu[g]], [self.bsgu])
            for n in range(8):
                j2, sv2, sbuf2 = sl_brb[n // 4]
                ps2, bps2 = self.next_ps()
                for k in range(4):
                    self.mm(ps2[:], sv2[:, k, (n % 4) * 128:(n % 4 + 1) * 128], self.sgu[:, k, :], k == 0, k == 3,
                            [sbuf2, self.bsgu], [bps2])
                tf, btf = self.tmpf[:, n % 2, :], self.btmpf[n % 2]
                self.tt(DVE, tf, ps2[:], self.sig[:, n, :], ALU.mult, [bps2, self.bsig[n]], [btf])
                self.tt(DVE, self.mg[:, t, n, :], self.mg[:, t, n, :], tf, ALU.add, [btf, self.bmg[t][n]], [self.bmg[t][n]])
        for wb in [wb_u, wb_v] + wb_gb:
            wb.done()
        for s in sl_brb:
            R.done(s[0])
        if self.stop_after == ("mixB", l):
            return
        wb_o = [WBlk(R, self.w_out[l], c * 512, 512) for c in range(2)]
        for t in range(NT):
            self.proj_accum_x(t, wb_o, lambda k, t=t: self.mg[:, t, k, :], [self.bmg[t][k] for k in range(8)], 8)
        for wb in wb_o:
            wb.done()

    def phase_xattn(self, l, p):
        P, R = self.P, self.R
        P.barrier(extra_bufs=[self.bofin])
        self.scratch("xat")
        wkv = self.w_xkv[l]
        for c2 in range(2):
            wb = WBlk(R, wkv, c2 * 512, 512)
            for c in range(4):
                n = c2 * 4 + c
                ps, bps = self.next_ps()
                for k in range(8):
                    wl, wbuf = wb.lhsT(k, c)
                    self.mm(ps[:, 0:MEM], wl, self.memn[:, k, :], k == 0, k == 7, [wbuf, self.bmemn], [bps])
                self.act(self.KT[:, n, :], ps[:, 0:MEM], AF.Copy, [bps], [self.bKT])
            wb.done()
        for c2 in range(2):
            wb = WBlk(R, wkv, D + c2 * 512, 512)
            for mc in range(2):
                ps, bps = self.next_ps()
                for k in range(8):
                    wr_, wbuf = wb.rows(k, 0, 512)
                    self.mm(ps[:], self.memn[:, k, mc * 128:(mc + 1) * 128], wr_, k == 0, k == 7, [wbuf, self.bmemn], [bps])
                self.act(self.V[:, mc, c2 * 512:(c2 + 1) * 512], ps[:], AF.Copy, [bps], [self.bV])
            wb.done()
        for t in range(NT):
            xi, bxi = self.xin_t(t)
            self.rmsnorm(xi, bxi, V_NXAT + 8 * l, lambda kc, t=t: self.h[:, t, kc, :], self.bh[t])
        wb_q = [WBlk(R, self.w_xq[l], c * 512, 512) for c in range(2)]
        for t in range(NT):
            for n in range(8):
                ps, bps = self.next_ps()
                for k in range(8):
                    wl, wbuf = wb_q[n // 4].lhsT(k, n % 4)
                    self.mm(ps[:], wl, self.h[:, t, k, :], k == 0, k == 7, [wbuf, self.bh[t]], [bps])
                self.act(self.mg[:, t, n, :], ps[:], AF.Identity, [bps], [self.bmg[t][n]], scale=1.0 / 16.0)
        for wb in wb_q:
            wb.done()
        for t in range(NT):
            for hd in range(4):
                r = hd % 2
                e_, be = self.eT[:, 2 * r:2 * r + 2, :], self.beT[r]
                for mc in range(2):
                    ps, bps = self.next_ps()
                    for dc in range(2):
                        n = hd * 2 + dc
                        self.mm(ps[:], self.KT[:, n, mc * 128:(mc + 1) * 128], self.mg[:, t, n, :], dc == 0, dc == 1,
                                [self.bKT, self.bmg[t][n]], [bps])
                    self.act(e_[:, mc, :], ps[:], AF.Exp, [bps], [be])
                ps, bps = self.next_ps()
                for mc in range(2):
                    self.mm(ps[:], self.ones_bf[:], e_[:, mc, :], mc == 0, mc == 1, [self.bones, be], [bps])
                rs, brs = self.rs[:, r, :], self.brs[r]
                self.P.op(DVE, lambda e, rs=rs, ps=ps: e.reciprocal(out=rs, in_=ps[:]), [bps], [brs])
                for dc in range(2):
                    n = hd * 2 + dc
                    ps, bps = self.next_ps()
                    for mc in range(2):
                        self.mm(ps[:], self.V[:, mc, n * 128:(n + 1) * 128], e_[:, mc, :], mc == 0, mc == 1, [self.bV, be], [bps])
                    self.tt(DVE, self.h[:, t, n, :], ps[:], rs, ALU.mult, [bps, brs], [self.bh[t]])
        if self.stop_after == ("xatO", l):
            return
        wb_o = [WBlk(R, self.w_xo[l], c * 512, 512) for c in range(2)]
        for t in range(NT):
            self.proj_accum_x(t, wb_o, lambda k, t=t: self.h[:, t, k, :], [self.bh[t]], 8)
        for wb in wb_o:
            wb.done()

    def router(self, t):
        for jc in range(4):
            j8 = t * 4 + jc
            r = jc % 2
            rt, brt = self.rt[:, 4 * r:4 * r + 4, :], self.brt[r]
            ps, bps = self.next_ps()
            for k in range(8):
                self.mm(ps[:, 0:NE], self.hf[:, k, jc * 128:(jc + 1) * 128], self.wr[:, k, :], k == 0, k == 7,
                        [self.bhf, self.bwr], [bps])
            lg = rt[:, 0, 0:8]
            self.cp(DVE, lg, ps[:, 0:NE], [bps], [brt])
            m1 = rt[:, 1, 0:1]
            self.P.op(DVE, lambda e, m1=m1, lg=lg: e.reduce_max(out=m1, in_=lg, axis=AX.X), [brt], [brt])
            eq = rt[:, 0, 8:16]
            self.ts(DVE, eq, lg, m1, None, ALU.is_equal, None, [brt], [brt])
            l2 = rt[:, 2, 0:8]
            self.stt(DVE, l2, eq, -1e30, lg, ALU.mult, ALU.add, [brt], [brt])
            m2 = rt[:, 1, 1:2]
            self.P.op(DVE, lambda e, m2=m2, l2=l2: e.reduce_max(out=m2, in_=l2, axis=AX.X), [brt], [brt])
            sel = self.sel_t[:, j8, :]
            self.ts(DVE, sel, lg, m2, None, ALU.is_ge, None, [brt], [brt, self.bselt[j8]])
            self.cp(DVE, self.selb_t[:, j8, :], sel, [self.bselt[j8]], [self.bselb[j8]])
            nm1 = rt[:, 1, 2:3]
            self.ts(DVE, nm1, m1, -1.0, None, ALU.mult, None, [brt], [brt])
            ex = rt[:, 3, 0:8]
            self.act(ex, lg, AF.Exp, [brt], [brt], bias=nm1, scale=1.0)
            exs = rt[:, 3, 8:16]
            self.tt(DVE, exs, ex, sel, ALU.mult, [brt, self.bselt[j8]], [brt])
            den = rt[:, 1, 3:4]
            self.P.op(DVE, lambda e, den=den, exs=exs: e.reduce_sum(out=den, in_=exs, axis=AX.X), [brt], [brt])
            rden = rt[:, 1, 4:5]
            self.P.op(DVE, lambda e, rden=rden, den=den: e.reciprocal(out=rden, in_=den), [brt], [brt])
            self.ts(DVE, self.comb_t[:, j8, :], exs, rden, None, ALU.mult, None, [brt], [self.bcomb[j8]])

    def ffn_expert(self, w_gu, w_down, dff, e=None):
        R = self.R
        g0 = 0
        gi = 0
        while g0 < dff:
            G = min(512, dff - g0)
            nch = G // 128
            wb_g = WBlk(R, w_gu, g0, G)
            wb_u = WBlk(R, w_gu, dff + g0, G)
            sl_d = [R.get(wsrc(w_down, g0, G, c * (SLAB // nch), SLAB // nch), nch) for c in range(D * nch // SLAB)]
            dcols = SLAB // nch
            for t in range(NT):
                a_i = (gi * NT + t) % 2
                actv, bact = self.actb[:, 4 * a_i:4 * a_i + 4, :], self.bact[a_i]
                if e is not None and gi == 0:
                    cb, bcb = self.cbc[:, t, :], self.bcbc[t]
                    ps, bps = self.next_ps()
                    for jc in range(4):
                        j8 = t * 4 + jc
                        cx, bcx = self.cexp[:, jc % 2, :], self.bcexp[jc % 2]
                        self.cp(DVE, cx, self.comb[:, j8, e:e + 1].to_broadcast([128, 128]), [self.bcomb[j8]], [bcx])
                        self.mm(ps[:, jc * 128:(jc + 1) * 128], cx, self.ident(), True, True, [bcx, self.bcst], [bps], inc=True)
                    self.act(cb, ps[:], AF.Copy, [bps], [bcb])
                for c in range(nch):
                    psg, bpsg = self.next_ps()
                    for k in range(8):
                        wl, wbuf = wb_g.lhsT(k, c)
                        self.mm(psg[:], wl, self.h[:, t, k, :], k == 0, k == 7, [wbuf, self.bh[t]], [bpsg])
                    psu, bpsu = self.next_ps()
                    for k in range(8):
                        wl, wbuf = wb_u.lhsT(k, c)
                        self.mm(psu[:], wl, self.h[:, t, k, :], k == 0, k == 7, [wbuf, self.bh[t]], [bpsu])
                    sg, bsg = self.sg[:, c % 2, :], self.bsg[c % 2]
                    self.act(sg, psg[:], AF.Silu, [bpsg], [bsg])
                    if e is not None:
                        self.tt(DVE, sg, sg, self.cbc[:, t, :], ALU.mult, [bsg, self.bcbc[t]], [bsg])
                    self.tt(DVE, actv[:, c, :], psu[:], sg, ALU.mult, [bpsu, bsg], [bact])
                n = 0
                for (jd, svd, sbd) in sl_d:
                    for cc in range(dcols // 128):
                        ps, bps = self.next_ps()
                        for k in range(nch):
                            self.mm(ps[:], svd[:, k, cc * 128:(cc + 1) * 128], actv[:, k, :], k == 0, k == nch - 1,
                                    [sbd, bact], [bps])
                        self.tt(DVE, self.x[:, t, n, :], self.x[:, t, n, :], ps[:], ALU.add, [bps, self.bx[t][n]], [self.bx[t][n]])
                        n += 1
            wb_g.done()
            wb_u.done()
            for s in sl_d:
                R.done(s[0])
            g0 += G
            gi += 1

    def bcast_tok(self, dst, bdst, src_t, bsrc, e):
        for half in range(2):
            ps, bps = self.next_ps()
            for jc in range(4):
                c = half * 4 + jc
                cx, bcx = self.cexp[:, jc % 2, :], self.bcexp[jc % 2]
                self.cp(DVE, cx, src_t[:, c, e:e + 1].to_broadcast([128, 128]), [bsrc[c]], [bcx])
                self.mm(ps[:, jc * 128:(jc + 1) * 128], cx, self.ident(), True, True, [bcx, self.bcst], [bps], inc=True)
            self.act(dst[:, half * TT:(half + 1) * TT], ps[:], AF.Copy, [bps], [bdst])

    def phase_moe(self, l, p):
        P, R = self.P, self.R
        P.barrier(extra_bufs=[self.bofin])
        self.scratch("moe_a")
        self.bcomb = [Buf("comb%d" % j) for j in range(8)]
        self.bselt = [Buf("selt%d" % j) for j in range(8)]
        self.bselb = [Buf("selb%d" % j) for j in range(8)]
        self.bposm = [Buf("posm%d" % j) for j in range(8)]
        self.bcntf, self.bcnti = Buf("cntf"), Buf("cnti")
        bhT = [Buf("hT%d" % c) for c in range(8)]
        hT = self.mg[:].rearrange("p t k n -> p (t k n)").rearrange("p (c f) -> p c f", c=8)
        for t in range(NT):
            xi, bxi = self.xin_t(t)
            self.rmsnorm(xi, bxi, V_NFFN + 8 * l, None, None, out_f32=lambda kc: self.hf[:, kc, :], bout_f32=self.bhf)
            self.router(t)
            for jc in range(4):
                c = t * 4 + jc
                for g in range(2):
                    ps, bps = self.next_ps()
                    for k4 in range(4):
                        fch = g * 4 + k4
                        self.P.op(PE, lambda e, ps=ps, k4=k4, fch=fch, jc=jc: e.transpose(
                            ps[:, k4 * 128:(k4 + 1) * 128], self.hf[:, fch, jc * 128:(jc + 1) * 128], self.ident()),
                            [self.bhf, self.bcst], [bps], inc=(k4 == 3))
                    self.act(hT[:, c, g * 512:(g + 1) * 512], ps[:], AF.Copy, [bps], [bhT[c]])
        for c in range(8):
            ps, bps = self.next_ps()
            for c2 in range(c):
                self.mm(ps[:, 0:NE], self.ones_bf[:], self.selb_t[:, c2, :], c2 == 0, False, [self.bones, self.bselb[c2]], [bps])
            self.mm(ps[:, 0:NE], self.U_bf[:], self.selb_t[:, c, :], c == 0, True, [self.bU, self.bselb[c]], [bps])
            self.stt(DVE, self.posm_t[:, c, :], ps[:, 0:NE], 1.0, self.sel_t[:, c, :], ALU.add, ALU.mult,
                     [bps, self.bselt[c]], [self.bposm[c]])
            self.ts(DVE, self.posm_t[:, c, :], self.posm_t[:, c, :], -1.0, None, ALU.add, None, [self.bposm[c]], [self.bposm[c]])
        ps, bps = self.next_ps()
        for c in range(8):
            self.mm(ps[:, 0:NE], self.ones_bf[:], self.selb_t[:, c, :], c == 0, c == 7, [self.bones, self.bselb[c]], [bps])
        self.cp(DVE, self.cntf_t[:], ps[:, 0:NE], [bps], [self.bcntf])
        self.cp(DVE, self.cnti_t[:], self.cntf_t[0:1, :], [self.bcntf], [self.bcnti])
        P.barrier(extra_bufs=[self.bofin])
        self.scratch("moe_b")
        wgu_all, wdn_all = self.w_moe_gu[l // 2], self.w_moe_down[l // 2]
        ngrp = DFE // 512
        for e in range(NE):
            w_gu, w_down = wgu_all[e], wdn_all[e]
            P.cond_load(self.cnti_t[0:1, e:e + 1], self.bcnti)
            self.bcast_tok(self.combT, self.bcombT, self.comb_t, self.bcomb, e)
            self.bcast_tok(self.posT, self.bposT, self.posm_t, self.bposm, e)
            for gi in range(ngrp):
                g0 = gi * 512
                wb_g = WBlk(R, w_gu, g0, 512)
                wb_u = WBlk(R, w_gu, DFE + g0, 512)
                sl_d = [R.get(wsrc(w_down, g0, 512, c * 512, 512), 4) for c in range(2)]
                for i in range(MAXG):
                    P.cond_begin(None, None, i * GSL + 1)
                    he = self.he[:, 8 * i:8 * i + 8, :]
                    if gi == 0:
                        selv = self.selbuf.rearrange("p (c n) -> p c n", c=8)
                        for c in range(8):
                            self.ts(DVE, selv[:, c, :], self.iota_row(), self.posm_t[:, c, e:e + 1], float(-i * GSL),
                                    ALU.subtract, ALU.is_equal, [self.bcst, self.bposm[c]], [self.bsel])
                        for f in range(8):
                            ps, bps = self.next_ps()
                            for c in range(8):
                                self.mm(ps[:, 0:GSL], hT[:, c, f * 128:(f + 1) * 128], selv[:, c, :], c == 0, c == 7,
                                        [bhT[c], self.bsel], [bps])
                            if f % 2 == 0:
                                self.act(he[:, f, :], ps[:, 0:GSL], AF.Copy, [bps], [self.bhe[i][f]])
                            else:
                                self.cp(DVE, he[:, f, :], ps[:, 0:GSL], [bps], [self.bhe[i][f]])
                    a_i = (gi * MAXG + i) % 2
                    actv, bact = self.actb[:, 4 * a_i:4 * a_i + 4, :], self.bact[a_i]
                    for c4 in range(4):
                        psg, bpsg = self.next_ps()
                        for k in range(8):
                            wl, wbuf = wb_g.lhsT(k, c4)
                            self.mm(psg[:], wl, he[:, k, :], k == 0, k == 7, [wbuf, self.bhe[i][k]], [bpsg])
                        psu, bpsu = self.next_ps()
                        for k in range(8):
                            wl, wbuf = wb_u.lhsT(k, c4)
                            self.mm(psu[:], wl, he[:, k, :], k == 0, k == 7, [wbuf, self.bhe[i][k]], [bpsu])
                        sg, bsg = self.sg[:, c4 % 2, :], self.bsg[c4 % 2]
                        self.act(sg, psg[:], AF.Silu, [bpsg], [bsg])
                        self.tt(DVE, actv[:, c4, :], psu[:], sg, ALU.mult, [bpsu, bsg], [bact])
                    for q in range(NQ):
                        for half in range(2):
                            jd, svd, sbd = sl_d[half]
                            ps, bps = self.next_ps()
                            for k in range(4):
                                self.mm(ps[:], actv[:, k, q * 128:(q + 1) * 128], svd[:, k, :], k == 0, k == 3, [bact, sbd], [bps])
                            dst = self.yacc_parts[i][:, q, half * 512:(half + 1) * 512]
                            if gi == 0:
                                self.act(dst, ps[:], AF.Copy, [bps], [self.byacc[i][q]])
                            else:
                                self.tt(DVE, dst, dst, ps[:], ALU.add, [bps, self.byacc[i][q]], [self.byacc[i][q]])
                    if gi == ngrp - 1:
                        selT = self.selbuf.rearrange("p (q n) -> p q n", q=NQ)
                        for q in range(NQ):
                            self.ts(DVE, selT[:, q, :], self.posT, self.iota_col(), float(i * GSL + q * 128),
                                    ALU.subtract, ALU.is_equal, [self.bposT, self.bcst], [self.bsel])
                            self.act(self.yb[:, q, :], self.yacc_parts[i][:, q, :], AF.Copy, [self.byacc[i][q]], [self.byb[q]])
                        for t in range(NT):
                            for f in range(8):
                                ps, bps = self.next_ps()
                                for q in range(NQ):
                                    self.mm(ps[:], self.yb[:, q, f * 128:(f + 1) * 128], selT[:, q, t * TT:(t + 1) * TT], q == 0, q == NQ - 1,
                                            [self.byb[q], self.bsel], [bps])
                                tmp, btmp = self.stmp[:, f % 2, :], self.bstmp[f % 2]
                                self.tt(DVE, tmp, ps[:], self.combT[:, t * TT:(t + 1) * TT], ALU.mult, [bps, self.bcombT], [btmp])
                                self.tt(DVE, self.x[:, t, f, :], self.x[:, t, f, :], tmp, ALU.add, [btmp, self.bx[t][f]], [self.bx[t][f]])
                    if i % 2 == 1:
                        P.cond_end()
                        P.cond_end()
                wb_g.done()
                wb_u.done()
                for s_ in sl_d:
                    R.done(s_[0])

    def phase_ffn(self, l, p):
        if l % 2 == 1:
            return self.phase_moe(l, p)
        P = self.P
        P.barrier(extra_bufs=[self.bofin])
        self.scratch("ffn")
        moe = (l % 2 == 1)
        for t in range(NT):
            xi, bxi = self.xin_t(t)
            if moe:
                self.rmsnorm(xi, bxi, V_NFFN + 8 * l, lambda kc, t=t: self.h[:, t, kc, :], self.bh[t],
                             out_f32=lambda kc: self.hf[:, kc, :], bout_f32=self.bhf)
                self.router(t)
            else:
                self.rmsnorm(xi, bxi, V_NFFN + 8 * l, lambda kc, t=t: self.h[:, t, kc, :], self.bh[t])
        if moe:
            for e in range(NE):
                self.ffn_expert(self.w_moe_gu[l // 2, e], self.w_moe_down[l // 2, e], DFE, e=e)
        else:
            self.ffn_expert(self.w_ff_gu[l // 2], self.w_ff_down[l // 2], DFF)

    def dump_x(self, p, bout):
        P = self.P
        for t in range(NT):
            tok0 = p * TP + t * TT
            P.dma(SP, self.outT[:, tok0:tok0 + TT].rearrange("(kc p) n -> p kc n", p=128), self.x[:, t],
                  reads=[self.bx[t][k] for k in range(8)])

    def phase_final(self, p):
        P = self.P
        P.barrier(extra_bufs=[self.bofin])
        self.scratch("fin")
        for t in range(NT):
            xi, bxi = self.xin_t(t)
            self.rmsnorm(xi, bxi, V_NFIN, None, None, out_f32=lambda kc: self.ofin[:, kc, :], bout_f32=self.bofin)
            tok0 = p * TP + t * TT
            P.dma(SP, self.outT[:, tok0:tok0 + TT].rearrange("(kc p) n -> p kc n", p=128), self.ofin, reads=[self.bofin])
            if p + 1 < self.n_pass:
                tok1 = (p + 1) * TP + t * TT
                P.dma(POOL, self.x[:, t], self.xT[:, tok1:tok1 + TT].rearrange("(kc p) n -> p kc n", p=128),
                      writes=[self.bx[t][k] for k in range(8)])
            self.x_prefetched = True

    def body(self, P, R):
        self.P, self.R = P, R
        self.ps_i = 0
        self.bofin = Buf("ofin")
        self.load_consts()
        self.compute_memn()
        self.x_prefetched = False
        for p in range(self.n_pass):
            for t in range(NT):
                if p > 0 and self.x_prefetched:
                    break
                tok0 = p * TP + t * TT
                P.dma(SP, self.x[:, t], self.xT[:, tok0:tok0 + TT].rearrange("(kc p) n -> p kc n", p=128),
                      writes=[self.bx[t][k] for k in range(8)])
            stopped = False
            for l in range(DEPTH):
                self.load_layer_consts(l)
                self.phase_mixer(l, p)
                if self.stop_after in (("mixA", l), ("mixB", l), ("mix", l)):
                    stopped = True
                    break
                self.phase_xattn(l, p)
                if self.stop_after in (("xatO", l), ("xat", l)):
                    stopped = True
                    break
                self.phase_ffn(l, p)
                if self.stop_after == ("ffn", l):
                    stopped = True
                    break
            if stopped:
                self.dump_x(p, None)
            else:
                self.phase_final(p)
        outs = [self.bofin] + [self.bx[t][k] for t in range(NT) for k in range(8)]
        P.final_wait(SP, outs)


V_NMIX = 0
V_NXAT = 16
V_NFFN = 32
V_PSC = 48
V_NMEM = 56
V_NFIN = 64
NVEC = 72


def build_nc(n_pass=NPASS, stop_after=None):
    nc = bass.Bass("TRN2", target_bir_lowering=False)
    with ExitStack() as st:
        B = Builder(nc, st, n_pass=n_pass, stop_after=stop_after)
        dryP = Prog(nc, st, dry=True)
        dryR = Ring(dryP, B.ring_t, plan=None)
        B.body(dryP, dryR)
        plan = dryR.plan
        P = Prog(nc, st)
        cache = None
        if n_pass > 1:
            assert len(plan) % n_pass == 0
            cache = nc.dram_tensor("wcache", [len(plan) // n_pass, 128, SLAB], BF16).ap()
        R = Ring(P, B.ring_t, plan=plan, cache=cache, n_pass=n_pass)
        B.body(P, R)
        assert R.idx == len(plan)
        P.emit_all()
    return nc


def _cols(v):
    v = np.asarray(v, np.float32)
    return np.ascontiguousarray(v.reshape(-1, 128).T)


def host_consts():
    c = np.zeros((128, 960), np.float32)
    c[:, 0:128] = np.eye(128, dtype=np.float32)
    s = np.arange(128)[:, None]
    t = np.arange(128)[None, :]
    c[:, 128:256] = (t >= s).astype(np.float32)
    for g in range(4):
        w = 2 << g
        tt = np.arange(16)
        c[:, 256 + g * 16:256 + (g + 1) * 16] = (w / np.minimum(tt + 1, w)).astype(np.float32)[None, :]
    c[:, 320:832] = np.arange(512, dtype=np.float32)[None, :]
    c[:, 832] = np.arange(128, dtype=np.float32)
    return c


def prep_shared(inp):
    f = lambda a: np.ascontiguousarray(np.asarray(a, np.float32))
    vec = np.zeros((128, NVEC), np.float32)
    for l in range(DEPTH):
        vec[:, V_NMIX + 8 * l:V_NMIX + 8 * l + 8] = _cols(inp["norm_mix"][l])
        vec[:, V_NXAT + 8 * l:V_NXAT + 8 * l + 8] = _cols(inp["norm_xattn"][l])
        vec[:, V_NFFN + 8 * l:V_NFFN + 8 * l + 8] = _cols(inp["norm_ffn"][l])
        vec[:, V_PSC + 4 * l:V_PSC + 4 * l + 4] = _cols(inp["pool_scale"][l])
    vec[:, V_NMEM:V_NMEM + 8] = _cols(inp["norm_mem"])
    vec[:, V_NFIN:V_NFIN + 8] = _cols(inp["norm_final"])
    bcs = np.zeros((DEPTH, 128, 1536), np.float32)
    for l in range(DEPTH):
        bcs[l, :, 0:512] = np.asarray(inp["sgu_ln_g"][l], np.float32)[None, :]
        bcs[l, :, 512:1024] = np.asarray(inp["sgu_ln_b"][l], np.float32)[None, :]
        bcs[l, :, 1024:1536] = np.asarray(inp["b_spatial"][l], np.float32).reshape(1, 512)
    sh = {
        "vecs": vec,
        "bcs": bcs,
        "cst": host_consts(),
        "w_in": f(inp["w_in"]),
        "pool_mix": f(np.asarray(inp["pool_mix"]).transpose(0, 2, 1, 3)),
        "wspT": f(np.asarray(inp["w_spatial"]).transpose(0, 3, 1, 2)),
        "w_branch_a": f(inp["w_branch_a"]),
        "w_branch_b": f(inp["w_branch_b"]),
        "w_out": f(inp["w_out"]),
        "w_xq": f(inp["w_xq"]),
        "w_xkv": f(inp["w_xkv"]),
        "w_xo": f(inp["w_xo"]),
        "w_ff_gu": f(inp["w_ff_gu"]),
        "w_ff_down": f(inp["w_ff_down"]),
        "w_router": f(np.asarray(inp["w_router"])[0].reshape(8, 128, NE).transpose(1, 0, 2)),
        "w_moe_gu": f(inp["w_moe_gu"]),
        "w_moe_down": f(inp["w_moe_down"]),
    }
    return sh


_NC_CACHE = {}


def kernel(**inputs):
    x = np.asarray(inputs["x"], np.float32)
    mem = np.asarray(inputs["mem"], np.float32)
    nb = x.shape[0]
    sh = prep_shared(inputs)
    in_maps = []
    for b in range(nb):
        m = dict(sh)
        m["xT"] = np.ascontiguousarray(x[b].T)
        m["memT"] = np.ascontiguousarray(mem[b].T)
        in_maps.append(m)
    if "nc" not in _NC_CACHE:
        _NC_CACHE["nc"] = build_nc()
    res = run_bass_kernel_spmd(_NC_CACHE["nc"], in_maps, core_ids=list(range(nb)))
    out = np.stack([np.ascontiguousarray(r["outT"].T) for r in res.results], axis=0)
    return out.astype(np.float32)
```
